# Optimizing a Trainium2 kernel written in Bass

```python
import math
import jax, jax.numpy as jnp
from jax import lax
import numpy as np

D_MODEL = 2048
BATCH = 1
SEQ = 8192
DEPTH = 2

GRID_W = 64
CTX_LEN = 256
EPS = 1e-6
N_ADA = 6
N_BRANCHES = 2

HY_WIDTH = D_MODEL
HY_SHORT = 3
HY_EMB_BANDS = 16
HY_EMB_DIM = 1 + 2 * HY_EMB_BANDS
HY_FILTER_HIDDEN = 64
HY_DECAY_TARGET = 1e-2
HY_FAST_DECAY = 0.3
HY_SLOW_DECAY = 1.5
HY_MIN_DECAY = math.log(HY_DECAY_TARGET) / HY_SLOW_DECAY
HY_MAX_DECAY = math.log(HY_DECAY_TARGET) / HY_FAST_DECAY

GLA_HEADS = 4
GLA_QK_WIDTH = D_MODEL // 2
GLA_V_WIDTH = D_MODEL
GLA_DK = GLA_QK_WIDTH // GLA_HEADS
GLA_DV = GLA_V_WIDTH // GLA_HEADS
GLA_GATE_RANK = 16
GLA_GATE_TEMP = 16.0
GLA_CHUNK = 64

COL_K = 0
COL_V = COL_K + GLA_QK_WIDTH
COL_AF = COL_V + GLA_V_WIDTH
COL_AB = COL_AF + GLA_GATE_RANK
STATE_COLS = COL_AB + GLA_GATE_RANK
COL_Q = STATE_COLS
COL_G = COL_Q + GLA_QK_WIDTH
COL_HY = COL_G + GLA_V_WIDTH
COL_GATE = COL_HY + 3 * HY_WIDTH
N_IN_COLS = COL_GATE + N_BRANCHES * D_MODEL

D_FF_DENSE = 128 * ((8 * D_MODEL // 3 + 127) // 128)
N_EXPERTS = 8
TOP_K = 2
D_FF_EXPERT = 7 * D_MODEL // 2

kernel_name = "hybrid_gla_hyena_moe_dit"

F32 = jnp.float32


def _rmsnorm(x, g):
    xf = x.astype(F32)
    xf = xf * lax.rsqrt(jnp.mean(xf * xf, axis=-1, keepdims=True) + EPS)
    return (xf * g.astype(F32)).astype(x.dtype)


def _modulation(cond, w, b):
    m = jax.nn.silu(cond) @ w + b
    return jnp.split(m, N_ADA, axis=-1)


def _modulate(x, g, shift, scale):
    return _rmsnorm(x, g) * (1 + scale) + shift


def _flip(a):
    return jnp.flip(a, axis=1)


def _gla_log_decay(a_low, w, b):
    B, L, _ = a_low.shape
    z = (a_low @ w + b).astype(F32)
    return (jax.nn.log_sigmoid(z) / GLA_GATE_TEMP).reshape(B, L, GLA_HEADS, GLA_DK)


def _gla_kv_decay(proj, p):
    B, L, _ = proj.shape
    k = proj[..., COL_K:COL_V].astype(F32).reshape(B, L, GLA_HEADS, GLA_DK)
    v = proj[..., COL_V:COL_AF].astype(F32).reshape(B, L, GLA_HEADS, GLA_DV)
    la_f = _gla_log_decay(proj[..., COL_AF:COL_AB], p["gla_aw_f"], p["gla_ab_f"])
    la_b = _gla_log_decay(proj[..., COL_AB:STATE_COLS], p["gla_aw_b"], p["gla_ab_b"])
    return k, v, la_f, la_b


def _gla_scan(q, k, v, loga, s0):
    B, L = q.shape[:2]
    n_chunks = L // GLA_CHUNK

    def to_chunks(a):
        return a.reshape(B, n_chunks, GLA_CHUNK, GLA_HEADS, -1).transpose(1, 0, 3, 2, 4)

    mask = jnp.tril(jnp.ones((GLA_CHUNK, GLA_CHUNK), bool))[:, :, None]

    def step(S, xs):
        qc, kc, vc, gc = xs
        b = jnp.cumsum(gc, axis=2)
        diff = b[:, :, :, None, :] - b[:, :, None, :, :]
        decay = jnp.exp(jnp.where(mask, diff, -jnp.inf))
        att = jnp.einsum("bhid,bhijd->bhij", qc, decay * kc[:, :, None])
        o = jnp.einsum("bhij,bhjv->bhiv", att, vc) + jnp.einsum("bhid,bhdv->bhiv", qc * jnp.exp(b), S)
        b_last = b[:, :, -1:, :]
        S_new = jnp.exp(b_last[:, :, 0, :])[..., None] * S + jnp.einsum(
            "bhjd,bhjv->bhdv", kc * jnp.exp(b_last - b), vc)
        return S_new, o

    s_fin, o = lax.scan(step, s0, (to_chunks(q), to_chunks(k), to_chunks(v), to_chunks(loga)))
    o = o.transpose(1, 0, 3, 2, 4).reshape(B, L, GLA_HEADS, GLA_DV)
    return o, s_fin


def _gla_final_state(k, v, loga):
    b = jnp.cumsum(loga, axis=1)
    return jnp.einsum("blhd,blhv->bhdv", k * jnp.exp(b[:, -1:] - b), v)


def _short_conv(u, w, b):
    L = u.shape[1]
    pad = HY_SHORT // 2
    up = jnp.pad(u, ((0, 0), (pad, pad), (0, 0)))
    out = b
    for j in range(HY_SHORT):
        out = out + up[:, j:j + L] * w[j]
    return out


def _hyena_filter(L, p):
    pos = jnp.arange(L, dtype=F32)
    t = pos / max(L - 1, 1)
    bands = jnp.linspace(1e-4, HY_EMB_BANDS - 1, HY_EMB_BANDS, dtype=F32)
    ang = (2.0 * math.pi / L) * pos[:, None] * bands[None, :]
    z = jnp.concatenate([t[:, None], jnp.cos(ang), -jnp.sin(ang)], axis=-1)
    freq = p["hy_freq"].astype(F32)
    h = jnp.sin(freq * (z @ p["hy_w1"].astype(F32) + p["hy_b1"].astype(F32)))
    h = jnp.sin(freq * (h @ p["hy_w2"].astype(F32) + p["hy_b2"].astype(F32)))
    h = jnp.sin(freq * (h @ p["hy_w3"].astype(F32) + p["hy_b3"].astype(F32)))
    h = h @ p["hy_w4"].astype(F32)
    deltas = jnp.abs(jnp.linspace(HY_MIN_DECAY, HY_MAX_DECAY, HY_WIDTH, dtype=F32))
    window = jnp.exp(-t[:, None] * deltas[None, :])
    h_fwd = h[:, :HY_WIDTH] * window
    h_bwd = h[:, HY_WIDTH:] * window
    filt = jnp.concatenate([h_fwd, jnp.zeros((1, HY_WIDTH), F32), h_bwd[:0:-1]], axis=0)
    return filt * lax.rsqrt(jnp.sum(filt * filt, axis=0, keepdims=True) + EPS)


def _hyena(u, p):
    B, L, _ = u.shape
    dt = u.dtype
    u = _short_conv(u, p["hy_conv_w"], p["hy_conv_b"])
    x0, x1, v = jnp.split(u, 3, axis=-1)
    filt = _hyena_filter(L, p)
    z = (x1 * v).astype(F32)
    zf = jnp.fft.rfft(z, n=2 * L, axis=1)
    hf = jnp.fft.rfft(filt, n=2 * L, axis=0)
    y = jnp.fft.irfft(zf * hf[None], n=2 * L, axis=1)[:, :L] + z * p["hy_bias"].astype(F32)
    return x0 * y.astype(dt)


def _token_mixer(h, s0_f, s0_b, p):
    B, L, _ = h.shape
    dt = h.dtype
    proj = h @ p["w_in"]
    k, v, la_f, la_b = _gla_kv_decay(proj, p)
    q = proj[..., COL_Q:COL_G].astype(F32).reshape(B, L, GLA_HEADS, GLA_DK) * (GLA_DK ** -0.5)
    o_f, s_f = _gla_scan(q, k, v, la_f, s0_f)
    o_b, s_b = _gla_scan(_flip(q), _flip(k), _flip(v), _flip(la_b), s0_b)
    o = _rmsnorm(o_f + _flip(o_b), p["gla_norm_g"]).astype(dt).reshape(B, L, GLA_V_WIDTH)
    o_gla = o * jax.nn.silu(proj[..., COL_G:COL_HY])
    o_hy = _hyena(proj[..., COL_HY:COL_GATE], p)
    gate_hy, gate_gla = jnp.split(jax.nn.sigmoid(proj[..., COL_GATE:]), N_BRANCHES, axis=-1)
    merged = gate_hy * (o_hy @ p["w_up_hy"]) + gate_gla * (o_gla @ p["w_up_gla"])
    return merged @ p["w_out"], s_f, s_b


def _context_states(h, p):
    proj = h @ p["w_in"][:, :STATE_COLS]
    k, v, la_f, la_b = _gla_kv_decay(proj, p)
    return _gla_final_state(k, v, la_f), _gla_final_state(_flip(k), _flip(v), _flip(la_b))


def _swiglu(h, wg, wu, wd):
    return (jax.nn.silu(h @ wg) * (h @ wu)) @ wd


def _moe(h, router_w, wg, wu, wd):
    logits = (h @ router_w).astype(F32)
    top_val, top_idx = lax.top_k(logits, TOP_K)
    top_w = jax.nn.softmax(top_val, axis=-1)
    gates = jnp.sum(jax.nn.one_hot(top_idx, N_EXPERTS, dtype=F32) * top_w[..., None], axis=-2).astype(h.dtype)
    y = jnp.zeros_like(h)
    for e in range(N_EXPERTS):
        y = y + gates[..., e:e + 1] * _swiglu(h, wg[e], wu[e], wd[e])
    return y


def setup_inputs(seed: int = 0) -> dict:
    key = jax.random.key(seed)
    ks = iter(jax.random.split(key, 48))

    def nrm(shape, scale):
        return jax.random.normal(next(ks), shape, jnp.float32) * scale

    D = D_MODEL
    W = HY_WIDTH
    n_dense = (DEPTH + 1) // 2
    n_moe = DEPTH // 2
    return {
        "x": nrm((BATCH, SEQ, D), 1.0),
        "c": nrm((BATCH, D), 1.0),
        "ctx": nrm((BATCH, CTX_LEN, D), 1.0),
        "c_ctx": nrm((D,), 1.0),
        "ada_w": nrm((DEPTH, D, N_ADA * D), 0.5 * D ** -0.5),
        "ada_b": nrm((DEPTH, N_ADA * D), 0.02),
        "norm_mix_g": 1.0 + nrm((DEPTH, D), 0.02),
        "norm_ffn_g": 1.0 + nrm((DEPTH, D), 0.02),
        "w_in": nrm((DEPTH, D, N_IN_COLS), D ** -0.5),
        "hy_conv_w": nrm((DEPTH, HY_SHORT, 3 * W), HY_SHORT ** -0.5),
        "hy_conv_b": nrm((DEPTH, 3 * W), 0.02),
        "hy_w1": nrm((DEPTH, HY_EMB_DIM, HY_FILTER_HIDDEN), HY_EMB_DIM ** -0.5),
        "hy_b1": nrm((DEPTH, HY_FILTER_HIDDEN), 0.02),
        "hy_w2": nrm((DEPTH, HY_FILTER_HIDDEN, HY_FILTER_HIDDEN), HY_FILTER_HIDDEN ** -0.5),
        "hy_b2": nrm((DEPTH, HY_FILTER_HIDDEN), 0.02),
        "hy_w3": nrm((DEPTH, HY_FILTER_HIDDEN, HY_FILTER_HIDDEN), HY_FILTER_HIDDEN ** -0.5),
        "hy_b3": nrm((DEPTH, HY_FILTER_HIDDEN), 0.02),
        "hy_w4": nrm((DEPTH, HY_FILTER_HIDDEN, 2 * W), HY_FILTER_HIDDEN ** -0.5),
        "hy_freq": 1.0 + nrm((DEPTH, HY_FILTER_HIDDEN), 0.02),
        "hy_bias": nrm((DEPTH, W), 0.5),
        "gla_aw_f": nrm((DEPTH, GLA_GATE_RANK, GLA_QK_WIDTH), GLA_GATE_RANK ** -0.5),
        "gla_ab_f": nrm((DEPTH, GLA_QK_WIDTH), 0.02),
        "gla_aw_b": nrm((DEPTH, GLA_GATE_RANK, GLA_QK_WIDTH), GLA_GATE_RANK ** -0.5),
        "gla_ab_b": nrm((DEPTH, GLA_QK_WIDTH), 0.02),
        "gla_norm_g": 1.0 + nrm((DEPTH, GLA_DV), 0.02),
        "w_up_hy": nrm((DEPTH, W, D), W ** -0.5),
        "w_up_gla": nrm((DEPTH, GLA_V_WIDTH, D), GLA_V_WIDTH ** -0.5),
        "w_out": nrm((DEPTH, D, D), D ** -0.5),
        "ffn_w_gate": nrm((n_dense, D, D_FF_DENSE), D ** -0.5),
        "ffn_w_up": nrm((n_dense, D, D_FF_DENSE), D ** -0.5),
        "ffn_w_down": nrm((n_dense, D_FF_DENSE, D), D_FF_DENSE ** -0.5),
        "router_w": nrm((n_moe, D, N_EXPERTS), D ** -0.5),
        "exp_w_gate": nrm((n_moe, N_EXPERTS, D, D_FF_EXPERT), D ** -0.5),
        "exp_w_up": nrm((n_moe, N_EXPERTS, D, D_FF_EXPERT), D ** -0.5),
        "exp_w_down": nrm((n_moe, N_EXPERTS, D_FF_EXPERT, D), D_FF_EXPERT ** -0.5),
        "final_norm_g": 1.0 + nrm((D,), 0.02),
    }


def reference(x, c, ctx, c_ctx, ada_w, ada_b, norm_mix_g, norm_ffn_g, w_in, hy_conv_w, hy_conv_b,
              hy_w1, hy_b1, hy_w2, hy_b2, hy_w3, hy_b3, hy_w4, hy_freq, hy_bias,
              gla_aw_f, gla_ab_f, gla_aw_b, gla_ab_b, gla_norm_g, w_up_hy, w_up_gla, w_out,
              ffn_w_gate, ffn_w_up, ffn_w_down, router_w, exp_w_gate, exp_w_up, exp_w_down,
              final_norm_g):
    B = x.shape[0]
    x_lat, x_ctx = x, ctx
    for l in range(DEPTH):
        last = l == DEPTH - 1
        p = {
            "w_in": w_in[l], "hy_conv_w": hy_conv_w[l], "hy_conv_b": hy_conv_b[l],
            "hy_w1": hy_w1[l], "hy_b1": hy_b1[l], "hy_w2": hy_w2[l], "hy_b2": hy_b2[l],
            "hy_w3": hy_w3[l], "hy_b3": hy_b3[l], "hy_w4": hy_w4[l], "hy_freq": hy_freq[l],
            "hy_bias": hy_bias[l], "gla_aw_f": gla_aw_f[l], "gla_ab_f": gla_ab_f[l],
            "gla_aw_b": gla_aw_b[l], "gla_ab_b": gla_ab_b[l], "gla_norm_g": gla_norm_g[l],
            "w_up_hy": w_up_hy[l], "w_up_gla": w_up_gla[l], "w_out": w_out[l],
        }
        lat_mod = [m[:, None, :] for m in _modulation(c, ada_w[l], ada_b[l])]
        ctx_mod = _modulation(c_ctx, ada_w[l], ada_b[l])

        h_lat = _modulate(x_lat, norm_mix_g[l], lat_mod[0], lat_mod[1])
        h_ctx = _modulate(x_ctx, norm_mix_g[l], ctx_mod[0], ctx_mod[1])
        if last:
            s_f, s_b = _context_states(h_ctx, p)
        else:
            zero_state = jnp.zeros((B, GLA_HEADS, GLA_DK, GLA_DV), F32)
            o_ctx, s_f, s_b = _token_mixer(h_ctx, zero_state, zero_state, p)
            x_ctx = x_ctx + ctx_mod[2] * o_ctx
        o_lat, _, _ = _token_mixer(h_lat, s_f, s_b, p)
        x_lat = x_lat + lat_mod[2] * o_lat

        if l % 2 == 0:
            i = l // 2
            ffn = lambda h, i=i: _swiglu(h, ffn_w_gate[i], ffn_w_up[i], ffn_w_down[i])
        else:
            i = l // 2
            ffn = lambda h, i=i: _moe(h, router_w[i], exp_w_gate[i], exp_w_up[i], exp_w_down[i])
        x_lat = x_lat + lat_mod[5] * ffn(_modulate(x_lat, norm_ffn_g[l], lat_mod[3], lat_mod[4]))
        if not last:
            x_ctx = x_ctx + ctx_mod[5] * ffn(_modulate(x_ctx, norm_ffn_g[l], ctx_mod[3], ctx_mod[4]))
    return _rmsnorm(x_lat, final_norm_g)
```

```python
import contextlib
import math
import numpy as np
import ml_dtypes
import concourse.bass as bass
import concourse.mybir as mybir
from concourse.bass_utils import run_bass_kernel_spmd

F32, BF16 = mybir.dt.float32, mybir.dt.bfloat16
AF, ALU, AX = mybir.ActivationFunctionType, mybir.AluOpType, mybir.AxisListType
NPBF = ml_dtypes.bfloat16
NCORES = 8
D = 2048
L = 8192
CTX = 256
T = L + CTX
KC = 16
EPS = 1e-6
DFF = 5504
DFE = 7168
NEXP = 8

COMPUTE = ("pe", "act", "dve", "pool")


def _is_psum_key(k):
    if isinstance(k, str):
        return k.startswith("p_") or k in ("pss", "psr")
    return isinstance(k, tuple) and k[0] in ("ps", "p_S")


class Op:
    __slots__ = ("eng", "fn", "reads", "writes", "dma", "deps", "sig", "idx", "nsem")

    def __init__(self, eng, fn, reads, writes, dma, nsem):
        self.eng, self.fn, self.reads, self.writes, self.dma = eng, fn, reads, writes, dma
        self.deps = set()
        self.sig = None
        self.nsem = nsem


class Prog:
    def __init__(self, nc, n_dma_sems=24):
        self.nc = nc
        self.ops = []
        self.n_dma_sems = n_dma_sems
        self.last_w = {}
        self.readers = {}

    def engine(self, e):
        nc = self.nc
        return {"pe": nc.tensor, "act": nc.scalar, "dve": nc.vector, "pool": nc.gpsimd,
                "sp": nc.sync, "gq": nc.gpsimd}[e]

    def add(self, eng, fn, r=(), w=(), dma=False, nsem=16):
        xr = [k for k in r if _is_psum_key(k)]
        if xr:
            w = tuple(w) + tuple(k for k in xr if k not in w)
        op = Op(eng, fn, tuple(r), tuple(w), dma, nsem)
        op.idx = len(self.ops)
        deps = op.deps
        for k in op.reads:
            lw = self.last_w.get(k)
            if lw is not None:
                deps.add(lw)
        for k in op.writes:
            lw = self.last_w.get(k)
            if lw is not None:
                deps.add(lw)
            rd = self.readers.get(k)
            if rd:
                for v in rd.values():
                    if isinstance(v, list):
                        deps.update(v)
                    else:
                        deps.add(v)
        deps.discard(op.idx)
        for k in op.writes:
            self.last_w[k] = op.idx
            self.readers[k] = {}
        for k in op.reads:
            rd = self.readers.setdefault(k, {})
            if dma:
                rd.setdefault(("dma", eng), []).append(op.idx)
            else:
                rd[eng] = op.idx
        self.ops.append(op)
        return op

    def emit(self):
        nc = self.nc
        ops = self.ops
        has_dep = [False] * len(ops)
        for op in ops:
            qe = "pool" if op.eng == "gq" else op.eng
            for d in op.deps:
                dop = ops[d]
                dq = "pool" if dop.eng == "gq" else dop.eng
                if (not dop.dma) and (not op.dma) and dq == qe and qe == "pe":
                    continue
                has_dep[d] = True
        for op in ops:
            if op.dma:
                has_dep[op.idx] = True
        with contextlib.ExitStack() as st:
            esem = {e: st.enter_context(nc.semaphore("s_" + e)) for e in COMPUTE}
            dsem = [st.enter_context(nc.semaphore("d_%d" % i)) for i in range(self.n_dma_sems)]
            ecount = {e: 0 for e in COMPUTE}
            dcount = [0] * self.n_dma_sems
            dnext = 0
            known = {q: {} for q in ("pe", "act", "dve", "pool", "sp")}
            for op in ops:
                q = "pool" if op.eng == "gq" else op.eng
                eng = self.engine(op.eng)
                need = {}
                for d in op.deps:
                    dop = ops[d]
                    if dop.sig is None:
                        continue
                    kind, sid, val = dop.sig
                    if kind == "e" and sid == q and q == "pe" and not op.dma:
                        continue
                    key = (kind, sid)
                    if need.get(key, 0) < val:
                        need[key] = val
                si = None
                if op.dma:
                    si = dnext
                    dnext = (dnext + 1) % self.n_dma_sems
                    if dcount[si] > 0:
                        key = ("d", si)
                        if need.get(key, 0) < dcount[si]:
                            need[key] = dcount[si]
                for key, val in need.items():
                    if known[q].get(key, 0) >= val:
                        continue
                    sem = esem[key[1]] if key[0] == "e" else dsem[key[1]]
                    eng.wait_ge(sem, val)
                    known[q][key] = val
                ins = op.fn(eng)
                if has_dep[op.idx]:
                    if op.dma:
                        dcount[si] += op.nsem
                        ins.then_inc(dsem[si], op.nsem)
                        op.sig = ("d", si, dcount[si])
                    else:
                        ecount[q] += 1
                        ins.then_inc(esem[q], 1)
                        op.sig = ("e", q, ecount[q])
            for si in range(self.n_dma_sems):
                if dcount[si] > known["sp"].get(("d", si), 0):
                    nc.sync.wait_ge(dsem[si], dcount[si])


class Bld:
    def __init__(self):
        self.nc = bass.Bass("TRN2", target_bir_lowering=False)
        self.P = Prog(self.nc)
        self.st = contextlib.ExitStack()
        self.nps = 0

    def din(self, name, shape, dt):
        return self.nc.dram_tensor(name, list(shape), dt, kind="ExternalInput").ap()

    def dout(self, name, shape, dt):
        return self.nc.dram_tensor(name, list(shape), dt, kind="ExternalOutput").ap()

    def sb(self, name, shape, dt):
        return self.st.enter_context(self.nc.sbuf_tensor("s_" + name, list(shape), dt))

    def psum(self):
        self.nps += 1
        return self.st.enter_context(self.nc.psum_tensor("ps%d" % self.nps, [128, 512], F32))

    def dma(self, out, in_, r=(), w=(), q="sp"):
        self.P.add(q, lambda e: e.dma_start(out=out, in_=in_), r=r, w=w, dma=True)

    def mm(self, out, lhsT, rhs, start, stop, r=(), w=()):
        self.P.add("pe", lambda e: e.matmul(out, lhsT=lhsT, rhs=rhs, start=start, stop=stop), r=r, w=w)

    def act(self, out, in_, func, r=(), w=(), **kw):
        self.P.add("act", lambda e: e.activation(out=out, in_=in_, func=func, **kw), r=r, w=w)

    def tt(self, eng, out, in0, in1, op, r=(), w=()):
        self.P.add(eng, lambda e: e.tensor_tensor(out=out, in0=in0, in1=in1, op=op), r=r, w=w)

    def ts(self, eng, out, in0, s1, s2, op0, op1=None, r=(), w=()):
        if op1 is None:
            self.P.add(eng, lambda e: e.tensor_scalar(out=out, in0=in0, scalar1=s1, scalar2=None, op0=op0), r=r, w=w)
        else:
            self.P.add(eng, lambda e: e.tensor_scalar(out=out, in0=in0, scalar1=s1, scalar2=s2, op0=op0, op1=op1), r=r, w=w)

    def stt(self, eng, out, in0, scalar, in1, op0, op1, r=(), w=()):
        eng = "dve"
        self.P.add(eng, lambda e: e.scalar_tensor_tensor(out=out, in0=in0, scalar=scalar, in1=in1, op0=op0, op1=op1), r=r, w=w)

    def copy(self, eng, out, in_, r=(), w=()):
        if eng == "act":
            self.P.add("act", lambda e: e.copy(out=out, in_=in_), r=r, w=w)
        else:
            self.P.add(eng, lambda e: e.tensor_copy(out=out, in_=in_), r=r, w=w)

    def memset(self, eng, ap, val, w=()):
        self.P.add(eng, lambda e: e.memset(ap, val), w=w)

    def finish(self):
        self.P.emit()
        self.st.close()
        return self.nc


def run(nc, in_maps):
    res = run_bass_kernel_spmd(nc, in_maps, core_ids=list(range(NCORES)))
    return res.results


def build_mod():
    b = Bld()
    w = b.din("w", [2, D, 1536], F32)
    bias = b.din("bias", [128, 2, 12], F32)
    cv = b.din("cv", [D, 2], F32)
    out = b.dout("mod", [2, 1536, 2], F32)
    wsb = b.sb("wsb", [128, KC, 1536], F32)
    cs = b.sb("cs", [128, KC, 2], F32)
    bs = b.sb("bs", [128, 2, 12], F32)
    res = b.sb("res", [128, 2, 12, 2], F32)
    ps = [b.psum() for _ in range(2)]
    b.dma(cs[:], cv.rearrange("(kc p) v -> p kc v", p=128), w=["cs"])
    b.dma(bs[:], bias, w=["bs"])
    b.act(cs[:], cs[:], AF.Silu, r=["cs"], w=["cs"])
    for l in range(2):
        for h in range(2):
            b.dma(wsb[:, h * 8:(h + 1) * 8, :], w[l, h * 1024:(h + 1) * 1024, :].rearrange("(kc p) n -> p kc n", p=128),
                  w=[("wsb", h)])
        for m in range(12):
            p = ps[m % 2]
            for kc in range(KC):
                b.mm(p[:, 0:2], wsb[:, kc, m * 128:(m + 1) * 128], cs[:, kc, :], kc == 0, kc == KC - 1,
                     r=[("wsb", kc // 8), "cs"], w=[("ps", m % 2)])
            b.ts("dve", res[:, l, m, :], p[:, 0:2], bs[:, l, m:m + 1], None, ALU.add,
                 r=[("ps", m % 2), "bs"], w=[("res", l)])
        b.dma(out[l].rearrange("(m p) v -> p m v", p=128), res[:, l], r=[("res", l)])
    return b.finish()


def build_rn(ntok, tiles, n_y, y_dt, h_dt, router=False, want_x=True):
    b = Bld()
    NTM = max(n for _, n, _ in tiles)
    xT = b.din("xT", [D, ntok], F32)
    ys = [b.din("y%d" % j, [D, ntok], y_dt) for j in range(n_y)]
    vec = b.din("vec", [D, 7], F32)
    hout = b.dout("h", [D, ntok], h_dt)
    xout = b.dout("xn", [D, ntok], F32) if want_x else None
    vs = b.sb("vs", [128, KC, 7], F32)
    AB = b.sb("AB", [128, KC, 4], F32)
    ones = b.sb("ones", [128, 128], F32)
    xs = [b.sb("xs%d" % i, [128, KC, NTM], F32) for i in range(2)]
    ysb = [b.sb("ysb%d" % j, [128, KC, NTM], y_dt) for j in range(n_y)]
    acc = b.sb("acc", [128, KC, NTM], F32) if n_y > 1 else None
    sq = [b.sb("sq%d" % i, [128, NTM], F32) for i in range(2)]
    rstd = b.sb("rstd", [128, NTM], F32)
    h32 = b.sb("h32", [128, KC, NTM], F32)
    hb = b.sb("hb", [128, KC, NTM], h_dt) if h_dt != F32 else None
    pss = b.psum()
    if router:
        rw = b.din("rw", [D, NEXP], F32)
        gout = b.dout("gates", [ntok, NEXP], F32)
        rws = b.sb("rws", [128, KC, NEXP], F32)
        psr = b.psum()
        lg = b.sb("lg", [128, NEXP], F32)
        t1 = b.sb("t1", [128, NEXP], F32)
        t2 = b.sb("t2", [128, NEXP], F32)
        m1 = b.sb("m1", [128, 4], F32)
        b.dma(rws[:], rw.rearrange("(kc p) e -> p kc e", p=128), w=["rws"])
    b.dma(vs[:], vec.rearrange("(kc p) v -> p kc v", p=128), w=["vs"])
    b.memset("pool", ones[:], 1.0, w=["ones"])
    for c in range(2):
        b.ts("dve", AB[:, :, 2 * c], vs[:, :, 3 + 2 * c], 1.0, None, ALU.add, r=["vs"], w=["AB"])
        b.tt("dve", AB[:, :, 2 * c], AB[:, :, 2 * c], vs[:, :, 2], ALU.mult, r=["AB", "vs"], w=["AB"])
        b.copy("dve", AB[:, :, 2 * c + 1], vs[:, :, 4 + 2 * c], r=["vs"], w=["AB"])
    for ti, (t0, n, is_ctx) in enumerate(tiles):
        x_ = xs[ti % 2]
        xk = ("xs", ti % 2)
        b.dma(x_[:, :, :n], xT[:, t0:t0 + n].rearrange("(kc p) n -> p kc n", p=128), w=[xk])
        for j in range(n_y):
            b.dma(ysb[j][:, :, :n], ys[j][:, t0:t0 + n].rearrange("(kc p) n -> p kc n", p=128), w=[("y", j)],
                  q="sp")
        gcol = 1 if is_ctx else 0
        if n_y >= 1:
            src = ysb[0]
            if n_y > 1:
                b.tt("dve", acc[:, :, :n], ysb[0][:, :, :n], ysb[1][:, :, :n], ALU.add, r=[("y", 0), ("y", 1)], w=["acc"])
                for j in range(2, n_y):
                    b.tt("pool" if j % 2 else "dve", acc[:, :, :n], acc[:, :, :n], ysb[j][:, :, :n], ALU.add,
                         r=["acc", ("y", j)], w=["acc"])
                src = acc
            for kc in range(KC):
                b.stt("dve" if kc % 2 else "pool", x_[:, kc, :n], src[:, kc, :n], vs[:, kc, gcol:gcol + 1], x_[:, kc, :n],
                      ALU.mult, ALU.add, r=["acc", ("y", 0), "vs", xk], w=[xk])
            if want_x:
                b.dma(xout[:, t0:t0 + n].rearrange("(kc p) n -> p kc n", p=128), x_[:, :, :n], r=[xk])
        for kc in range(KC):
            s = sq[kc % 2]
            b.act(s[:, :n], x_[:, kc, :n], AF.Square, r=[xk], w=[("sq", kc % 2)])
            b.mm(pss[:, :n], ones[:], s[:, :n], kc == 0, kc == KC - 1, r=["ones", ("sq", kc % 2)], w=["pss"])
        b.act(rstd[:, :n], pss[:, :n], AF.Ln, r=["pss"], w=["rstd"], bias=EPS, scale=1.0 / D)
        b.act(rstd[:, :n], rstd[:, :n], AF.Exp, r=["rstd"], w=["rstd"], scale=-0.5)
        a0 = 2 if is_ctx else 0
        for kc in range(KC):
            e = "dve" if kc % 2 else "pool"
            b.tt(e, h32[:, kc, :n], x_[:, kc, :n], rstd[:, :n], ALU.mult, r=[xk, "rstd"], w=[("h32", kc)])
            dst = h32 if hb is None else hb
            b.ts(e, dst[:, kc, :n], h32[:, kc, :n], AB[:, kc, a0:a0 + 1], AB[:, kc, a0 + 1:a0 + 2], ALU.mult, ALU.add,
                 r=[("h32", kc), "AB"], w=[("hb", kc)])
        if router:
            for kc in range(KC):
                e = "dve" if kc % 2 else "pool"
                b.ts(e, h32[:, kc, :n], h32[:, kc, :n], AB[:, kc, a0:a0 + 1], AB[:, kc, a0 + 1:a0 + 2], ALU.mult, ALU.add,
                     r=[("h32", kc), ("hb", kc), "AB"], w=[("h32", kc)])
            for s0 in range(0, n, 128):
                m = min(128, n - s0)
                for kc in range(KC):
                    b.mm(psr[:m, 0:NEXP], h32[:, kc, s0:s0 + m], rws[:, kc, :], kc == 0, kc == KC - 1,
                         r=[("h32", kc), "rws"], w=["psr"])
                b.copy("dve", lg[:m], psr[:m, 0:NEXP], r=["psr"], w=["lg"])
                V = lambda *a, **k: b.P.add("dve", *a, **k)
                V(lambda e, m=m: e.tensor_reduce(out=m1[:m, 0:1], in_=lg[:m], axis=AX.X, op=ALU.max), r=["lg"], w=["m1"])
                b.ts("dve", t1[:m], lg[:m], m1[:m, 0:1], None, ALU.is_equal, r=["lg", "m1"], w=["t1"])
                b.stt("dve", t1[:m], t1[:m], -1e30, lg[:m], ALU.mult, ALU.add, r=["t1", "lg"], w=["t1"])
                V(lambda e, m=m: e.tensor_reduce(out=m1[:m, 1:2], in_=t1[:m], axis=AX.X, op=ALU.max), r=["t1"], w=["m1"])
                b.ts("dve", t1[:m], lg[:m], m1[:m, 1:2], None, ALU.is_ge, r=["lg", "m1"], w=["t1"])
                b.ts("dve", m1[:m, 2:3], m1[:m, 0:1], -1.0, None, ALU.mult, r=["m1"], w=["m1"])
                b.act(t2[:m], lg[:m], AF.Exp, r=["lg", "m1"], w=["t2"], bias=m1[:m, 2:3], scale=1.0)
                b.tt("dve", t2[:m], t2[:m], t1[:m], ALU.mult, r=["t1", "t2"], w=["t2"])
                V(lambda e, m=m: e.tensor_reduce(out=m1[:m, 3:4], in_=t2[:m], axis=AX.X, op=ALU.add), r=["t2"], w=["m1"])
                V(lambda e, m=m: e.reciprocal(out=m1[:m, 3:4], in_=m1[:m, 3:4]), r=["m1"], w=["m1"])
                b.ts("dve", t2[:m], t2[:m], m1[:m, 3:4], None, ALU.mult, r=["t2", "m1"], w=["t2"])
                b.dma(gout[t0 + s0:t0 + s0 + m, :], t2[:m], r=["t2"])
        dst = h32 if hb is None else hb
        b.dma(hout[:, t0:t0 + n].rearrange("(kc p) n -> p kc n", p=128), dst[:, :, :n],
              r=[("hb", kc) for kc in range(KC)] + [("h32", kc) for kc in range(KC)])
    return b.finish()


def run_mod(inp):
    nc = build_mod()
    cv = np.ascontiguousarray(np.stack([inp["c"][0], inp["c_ctx"]], 1))
    maps = []
    for c in range(NCORES):
        cols = np.concatenate([np.arange(k * D + 256 * c, k * D + 256 * c + 256) for k in range(6)])
        maps.append({"w": np.ascontiguousarray(inp["ada_w"][:, :, cols]),
                     "bias": np.ascontiguousarray(inp["ada_b"][:, cols].reshape(2, 12, 128).transpose(2, 0, 1)), "cv": cv})
    res = run(nc, maps)
    mod = np.zeros((2, 6, D, 2), np.float32)
    for c in range(NCORES):
        r = res[c]["mod"].reshape(2, 6, 256, 2)
        mod[:, :, 256 * c:256 * c + 256, :] = r
    return mod


def vec_for(mod, l, which, g):
    k0 = 0 if which == "mix" else 3
    if which == "final":
        z = np.zeros(D, np.float32)
        return np.ascontiguousarray(np.stack([mod[l, 5, :, 0], mod[l, 5, :, 1], g, z, z, z, z], 1))
    if which == "mix":
        gl, gc = (mod[l - 1, 5, :, 0], mod[l - 1, 5, :, 1]) if l > 0 else (np.zeros(D, np.float32),) * 2
    else:
        gl, gc = mod[l, 2, :, 0], mod[l, 2, :, 1]
    return np.ascontiguousarray(np.stack([gl, gc, g, mod[l, k0 + 1, :, 0], mod[l, k0, :, 0],
                                          mod[l, k0 + 1, :, 1], mod[l, k0, :, 1]], 1))


def tok_cols(c):
    return np.concatenate([np.arange(1024 * c, 1024 * c + 1024), np.arange(L + 32 * c, L + 32 * c + 32)])


_RN_CACHE = {}


def run_rn(xT, ys, y_dt, vec, h_dt, router_w=None, NT=512, want_x=True):
    ntok = 1056
    tiles = [(t0, min(NT, 1024 - t0), False) for t0 in range(0, 1024, NT)] + [(1024, 32, True)]
    nc = build_rn(ntok, tiles, len(ys), y_dt, h_dt, router=router_w is not None, want_x=want_x)
    maps = []
    for c in range(NCORES):
        cols = tok_cols(c)
        m = {"xT": np.ascontiguousarray(xT[:, cols]), "vec": vec}
        for j, y in enumerate(ys):
            m["y%d" % j] = np.ascontiguousarray(y[:, cols])
        if router_w is not None:
            m["rw"] = np.ascontiguousarray(router_w)
        maps.append(m)
    res = run(nc, maps)
    h = np.zeros((D, T), res[0]["h"].dtype)
    xn = np.zeros((D, T), np.float32) if want_x else None
    gates = np.zeros((T, NEXP), np.float32) if router_w is not None else None
    for c in range(NCORES):
        cols = tok_cols(c)
        h[:, cols] = res[c]["h"]
        if want_x:
            xn[:, cols] = res[c]["xn"]
        if gates is not None:
            gates[cols] = res[c]["gates"]
    return h, xn, gates


TT = [(t0, 512) for t0 in range(0, L, 512)] + [(L, CTX)]


class ActStream:
    def __init__(self, b, name, dram, KCn, dt=BF16, nbuf=2, nmax=512):
        self.b, self.name, self.dram, self.KCn, self.nbuf = b, name, dram, KCn, nbuf
        self.bufs = [b.sb("%s_%d" % (name, i), [128, KCn, nmax], dt) for i in range(nbuf)]

    def load(self, ti, t0, n, q="sp"):
        buf = self.bufs[ti % self.nbuf]
        key = (self.name, ti % self.nbuf)
        K = self.dram.shape[0]
        full = (K // 128) * 128
        if full:
            self.b.dma(buf[:, :K // 128, :n], self.dram[0:full, t0:t0 + n].rearrange("(kc p) n -> p kc n", p=128), w=[key], q=q)
        if K > full:
            self.b.dma(buf[:K - full, K // 128, :n], self.dram[full:K, t0:t0 + n], w=[key], q=q)
        return buf, key


def load_w(b, name, dram, K, ncols):
    KCn = K // 128
    wsb = b.sb(name, [128, KCn, ncols], BF16)
    step = max(1, 8192 // ncols)
    for k0 in range(0, KCn, step):
        k1 = min(KCn, k0 + step)
        b.dma(wsb[:, k0:k1, :], dram[k0 * 128:k1 * 128, :].rearrange("(kc p) n -> p kc n", p=128), w=[(name, k0)], q="gq")
    keys = [(name, k0) for k0 in range(0, KCn, step)]
    return wsb, keys, step


def build_out():
    b = Bld()
    mT = b.din("mT", [D, T], BF16)
    w = b.din("w", [D, 256], F32)
    o = b.dout("o", [256, T], F32)
    wsb, wk, ws = load_w(b, "w", w, D, 256)
    A = ActStream(b, "a", mT, KC)
    ps = [b.psum() for _ in range(4)]
    osb = [b.sb("osb%d" % i, [128, 512], F32) for i in range(4)]
    i = 0
    for ti, (t0, n) in enumerate(TT):
        a, ak = A.load(ti, t0, n)
        for m in range(2):
            p, pk = ps[i % 4], ("ps", i % 4)
            for kc in range(KC):
                b.mm(p[:, :n], wsb[:, kc, m * 128:(m + 1) * 128], a[:, kc, :n], kc == 0, kc == KC - 1,
                     r=[wk[kc // ws], ak], w=[pk])
            b.copy("act" if i % 2 else "dve", osb[i % 4][:, :n], p[:, :n], r=[pk], w=[("osb", i % 4)])
            b.dma(o[m * 128:(m + 1) * 128, t0:t0 + n], osb[i % 4][:, :n], r=[("osb", i % 4)])
            i += 1
    return b.finish()


def build_ffa():
    b = Bld()
    hT = b.din("hT", [D, T], BF16)
    wg = b.din("wg", [D, 688], F32)
    wu = b.din("wu", [D, 688], F32)
    o = b.dout("o", [688, T], BF16)
    wgs, gk, gs = load_w(b, "wg", wg, D, 688)
    wus, uk, us = load_w(b, "wu", wu, D, 688)
    A = ActStream(b, "a", hT, KC)
    ps = [b.psum() for _ in range(6)]
    sg = [b.sb("sg%d" % i, [128, 512], F32) for i in range(3)]
    ob = [b.sb("ob%d" % i, [128, 512], BF16) for i in range(3)]
    chunks = [(m * 128, 128) for m in range(5)] + [(640, 48)]
    i = 0
    for ti, (t0, n) in enumerate(TT):
        a, ak = A.load(ti, t0, n)
        for (c0, cn) in chunks:
            j = i % 3
            pg, pu = ps[2 * j], ps[2 * j + 1]
            for kc in range(KC):
                b.mm(pg[:cn, :n], wgs[:, kc, c0:c0 + cn], a[:, kc, :n], kc == 0, kc == KC - 1, r=[gk[kc // gs], ak], w=[("ps", 2 * j)])
            for kc in range(KC):
                b.mm(pu[:cn, :n], wus[:, kc, c0:c0 + cn], a[:, kc, :n], kc == 0, kc == KC - 1, r=[uk[kc // us], ak], w=[("ps", 2 * j + 1)])
            b.act(sg[j][:cn, :n], pg[:cn, :n], AF.Silu, r=[("ps", 2 * j)], w=[("sg", j)])
            b.tt("dve", ob[j][:cn, :n], sg[j][:cn, :n], pu[:cn, :n], ALU.mult, r=[("sg", j), ("ps", 2 * j + 1)], w=[("ob", j)])
            b.dma(o[c0:c0 + cn, t0:t0 + n], ob[j][:cn, :n], r=[("ob", j)])
            i += 1
    return b.finish()


def build_ffb():
    b = Bld()
    KCn = DFF // 128
    aT = b.din("aT", [DFF, T], BF16)
    w = b.din("w", [DFF, 256], F32)
    o = b.dout("o", [256, T], F32)
    wsb, wk, ws = load_w(b, "w", w, DFF, 256)
    A = ActStream(b, "a", aT, KCn)
    ps = [b.psum() for _ in range(4)]
    osb = [b.sb("osb%d" % i, [128, 512], F32) for i in range(4)]
    i = 0
    for ti, (t0, n) in enumerate(TT):
        a, ak = A.load(ti, t0, n)
        for m in range(2):
            p, pk = ps[i % 4], ("ps", i % 4)
            for kc in range(KCn):
                b.mm(p[:, :n], wsb[:, kc, m * 128:(m + 1) * 128], a[:, kc, :n], kc == 0, kc == KCn - 1,
                     r=[wk[kc // ws], ak], w=[pk])
            b.copy("act" if i % 2 else "dve", osb[i % 4][:, :n], p[:, :n], r=[pk], w=[("osb", i % 4)])
            b.dma(o[m * 128:(m + 1) * 128, t0:t0 + n], osb[i % 4][:, :n], r=[("osb", i % 4)])
            i += 1
    return b.finish()


def build_post():
    b = Bld()
    ohT = b.din("ohT", [D, T], BF16)
    ogT = b.din("ogT", [D, T], BF16)
    gT = b.din("gT", [512, T], BF16)
    wh = b.din("wh", [D, 256], F32)
    wg = b.din("wg", [D, 256], F32)
    ssq = b.din("ssq", [D, 2], F32)
    o = b.dout("o", [256, T], BF16)
    w32 = b.sb("w32", [128, KC, 256], F32)
    whs = [b.sb("whs%d" % i, [128, KC, 256], BF16) for i in range(2)]
    sv = b.sb("sv", [128, KC, 2], F32)
    b.dma(w32[:], wh.rearrange("(kc p) n -> p kc n", p=128), w=["w32"])
    b.dma(sv[:], ssq.rearrange("(kc p) v -> p kc v", p=128), w=["sv"])
    b.act(sv[:], sv[:], AF.Ln, r=["sv"], w=["sv"], bias=EPS, scale=1.0)
    b.act(sv[:], sv[:], AF.Exp, r=["sv"], w=["sv"], scale=-0.5)
    for v in range(2):
        for kc in range(KC):
            b.ts("dve" if kc % 2 else "pool", whs[v][:, kc, :], w32[:, kc, :], sv[:, kc, v:v + 1], None, ALU.mult,
                 r=["w32", "sv"], w=[("whs", v)])
    wgs, gk, gs = load_w(b, "wg", wg, D, 256)
    AH = ActStream(b, "ah", ohT, KC)
    AG = ActStream(b, "ag", ogT, KC)
    GT = ActStream(b, "gt", gT, 4)
    ps = [b.psum() for _ in range(4)]
    t1 = [b.sb("t1%d" % i, [128, 512], F32) for i in range(2)]
    t2 = [b.sb("t2%d" % i, [128, 512], F32) for i in range(2)]
    ob = [b.sb("ob%d" % i, [128, 512], BF16) for i in range(2)]
    i = 0
    for ti, (t0, n) in enumerate(TT):
        ah, ahk = AH.load(ti, t0, n)
        ag, agk = AG.load(ti, t0, n)
        gt, gtk = GT.load(ti, t0, n)
        v = 1 if t0 >= L else 0
        for m in range(2):
            j = i % 2
            p1, p2 = ps[2 * j], ps[2 * j + 1]
            for kc in range(KC):
                b.mm(p1[:, :n], whs[v][:, kc, m * 128:(m + 1) * 128], ah[:, kc, :n], kc == 0, kc == KC - 1,
                     r=[("whs", v), ahk], w=[("ps", 2 * j)])
            for kc in range(KC):
                b.mm(p2[:, :n], wgs[:, kc, m * 128:(m + 1) * 128], ag[:, kc, :n], kc == 0, kc == KC - 1,
                     r=[gk[kc // gs], agk], w=[("ps", 2 * j + 1)])
            b.tt("dve", t1[j][:, :n], p1[:, :n], gt[:, m, :n], ALU.mult, r=[("ps", 2 * j), gtk], w=[("t1", j)])
            b.tt("dve", t2[j][:, :n], p2[:, :n], gt[:, 2 + m, :n], ALU.mult, r=[("ps", 2 * j + 1), gtk], w=[("t2", j)])
            b.tt("pool", ob[j][:, :n], t1[j][:, :n], t2[j][:, :n], ALU.add, r=[("t1", j), ("t2", j)], w=[("ob", j)])
            b.dma(o[m * 128:(m + 1) * 128, t0:t0 + n], ob[j][:, :n], r=[("ob", j)])
            i += 1
    return b.finish()


def _gather_cols(res, name, rows):
    return np.concatenate([res[c][name] for c in range(NCORES)], 0)


def run_out(mT, w_out):
    nc = build_out()
    maps = [{"mT": mT, "w": np.ascontiguousarray(w_out[:, 256 * c:256 * c + 256])} for c in range(NCORES)]
    return _gather_cols(run(nc, maps), "o", 256)


def run_ffn_dense(hT, wg, wu, wd):
    nc = build_ffa()
    maps = [{"hT": hT, "wg": np.ascontiguousarray(wg[:, 688 * c:688 * c + 688]),
             "wu": np.ascontiguousarray(wu[:, 688 * c:688 * c + 688])} for c in range(NCORES)]
    aT = _gather_cols(run(nc, maps), "o", 688)
    nc = build_ffb()
    maps = [{"aT": aT, "w": np.ascontiguousarray(wd[:, 256 * c:256 * c + 256])} for c in range(NCORES)]
    return _gather_cols(run(nc, maps), "o", 256)


def run_post(ohT, ogT, gates_sh, w_up_hy, w_up_gla, ssq):
    nc = build_post()
    maps = [{"ohT": ohT, "ogT": ogT, "gT": gates_sh[c], "wh": np.ascontiguousarray(w_up_hy[:, 256 * c:256 * c + 256]),
             "wg": np.ascontiguousarray(w_up_gla[:, 256 * c:256 * c + 256]), "ssq": ssq} for c in range(NCORES)]
    return _gather_cols(run(nc, maps), "o", 256)


def build_hy():
    b = Bld()
    hT = b.din("hT", [D, T], BF16)
    w = b.din("w", [D, 768], F32)
    cw = b.din("cw", [768, 4], F32)
    x0o = b.dout("x0", [256, T], BF16)
    zo = b.dout("z", [256, T], BF16)
    wsb, wk, ws = load_w(b, "w", w, D, 768)
    cws = b.sb("cws", [128, 6, 4], F32)
    b.dma(cws[:], cw.rearrange("(m p) v -> p m v", p=128), w=["cws"])
    u = b.sb("u", [128, 6, T], BF16)
    A = ActStream(b, "a", hT, KC)
    ps = [b.psum() for _ in range(4)]
    i = 0
    for ti, (t0, n) in enumerate(TT):
        a, ak = A.load(ti, t0, n)
        for m in range(6):
            p, pk = ps[i % 4], ("ps", i % 4)
            for kc in range(KC):
                b.mm(p[:, :n], wsb[:, kc, m * 128:(m + 1) * 128], a[:, kc, :n], kc == 0, kc == KC - 1,
                     r=[wk[kc // ws], ak], w=[pk])
            b.copy("act" if i % 2 else "dve", u[:, m, t0:t0 + n], p[:, :n], r=[pk], w=[("u", m, ti)])
            i += 1
    SEG = 1024
    segs = [(s, s + SEG, 0, L) for s in range(0, L, SEG)] + [(L, T, L, T)]
    cA = [b.sb("cA%d" % i, [128, SEG], F32) for i in range(2)]
    cB = [b.sb("cB%d" % i, [128, SEG], F32) for i in range(2)]
    ob = [b.sb("cob%d" % i, [128, SEG], BF16) for i in range(4)]
    oi = 0

    def conv(dst, dk, m, s0, s1, lo, hi, eng):
        n = s1 - s0
        tiles = range(len(TT))
        rk = [("u", m, ti) for ti in tiles if TT[ti][0] < s1 + 1 and TT[ti][0] + TT[ti][1] > s0 - 1]
        b.ts(eng, dst[:, :n], u[:, m, s0:s1], cws[:, m, 1:2], cws[:, m, 3:4], ALU.mult, ALU.add, r=rk + ["cws"], w=[dk])
        if s0 > lo:
            b.stt(eng, dst[:, :n], u[:, m, s0 - 1:s1 - 1], cws[:, m, 0:1], dst[:, :n], ALU.mult, ALU.add, r=rk + ["cws", dk], w=[dk])
        else:
            b.stt(eng, dst[:, 1:n], u[:, m, s0:s1 - 1], cws[:, m, 0:1], dst[:, 1:n], ALU.mult, ALU.add, r=rk + ["cws", dk], w=[dk])
        if s1 < hi:
            b.stt(eng, dst[:, :n], u[:, m, s0 + 1:s1 + 1], cws[:, m, 2:3], dst[:, :n], ALU.mult, ALU.add, r=rk + ["cws", dk], w=[dk])
        else:
            b.stt(eng, dst[:, :n - 1], u[:, m, s0 + 1:s1], cws[:, m, 2:3], dst[:, :n - 1], ALU.mult, ALU.add, r=rk + ["cws", dk], w=[dk])

    for si, (s0, s1, lo, hi) in enumerate(segs):
        n = s1 - s0
        for j in range(2):
            q = (2 * si + j) % 2
            conv(cA[q], ("cA", q), 2 + j, s0, s1, lo, hi, "dve")
            conv(cB[q], ("cB", q), 4 + j, s0, s1, lo, hi, "pool")
            o1 = ob[oi % 4]
            b.tt("dve", o1[:, :n], cA[q][:, :n], cB[q][:, :n], ALU.mult, r=[("cA", q), ("cB", q)], w=[("ob", oi % 4)])
            b.dma(zo[j * 128:(j + 1) * 128, s0:s1], o1[:, :n], r=[("ob", oi % 4)])
            oi += 1
            conv(cA[q], ("cA", q), j, s0, s1, lo, hi, "pool")
            o2 = ob[oi % 4]
            b.copy("act", o2[:, :n], cA[q][:, :n], r=[("cA", q)], w=[("ob", oi % 4)])
            b.dma(x0o[j * 128:(j + 1) * 128, s0:s1], o2[:, :n], r=[("ob", oi % 4)])
            oi += 1
    return b.finish()


def run_hy(hT, w_in_l, conv_w, conv_b):
    nc = build_hy()
    maps = []
    COL_HY = 6176
    for c in range(NCORES):
        cols = np.concatenate([np.arange(COL_HY + j * D + 256 * c, COL_HY + j * D + 256 * c + 256) for j in range(3)])
        ccols = cols - COL_HY
        cw = np.concatenate([conv_w[:, ccols].T, conv_b[ccols][:, None]], 1)
        maps.append({"hT": hT, "w": np.ascontiguousarray(w_in_l[:, cols]), "cw": np.ascontiguousarray(cw.astype(np.float32))})
    res = run(nc, maps)
    return _gather_cols(res, "x0", 256), _gather_cols(res, "z", 256)


NF = 16384
GC = 32
HY_EMB_BANDS = 16


def fft_consts():
    a = np.arange(128)
    th = 2 * np.pi * np.outer(a, a) / 128.0
    c, s = np.cos(th), np.sin(th)
    ph = 2 * np.pi * np.outer(a, a) / NF
    bf = lambda x: np.ascontiguousarray(x.astype(np.float32).astype(NPBF))
    tw = np.stack([np.cos(ph), -np.sin(ph)], 1)
    tw8 = np.ascontiguousarray(np.repeat(tw[:, :, None, :], 8, 2).astype(np.float32))
    return {"FrFi": bf(np.concatenate([c, -s], 1)), "Fr": bf(c), "Fi": bf(-s), "nFi": bf(s),
            "GrGi": bf(np.concatenate([c, s], 1)), "nGiGr": bf(np.concatenate([-s, c], 1)),
            "nGi": bf(-s), "tw8": tw8}


def filter_tables(Lseq):
    m = np.arange(NF)
    pos = np.where(m < NF // 2, m, NF - m).astype(np.float64)
    valid = np.where(m < NF // 2, m < Lseq, (NF - m) < Lseq) & (m != NF // 2)
    valid &= ~((m >= NF // 2) & (pos == 0))
    t = pos / max(Lseq - 1, 1)
    bands = np.linspace(1e-4, HY_EMB_BANDS - 1, HY_EMB_BANDS)
    ang = (2.0 * math.pi / Lseq) * pos[:, None] * bands[None, :]
    zemb = np.concatenate([t[:, None], np.cos(ang), -np.sin(ang)], -1).T
    zemb = np.where(valid[None, :], zemb, 0.0)
    tneg = np.where(valid, -t, -1e4).reshape(128, 128)
    return np.ascontiguousarray(zemb.astype(np.float32)), np.ascontiguousarray(tneg.astype(np.float32))


def hy_deltas():
    mn, mx = math.log(1e-2) / 1.5, math.log(1e-2) / 0.3
    return np.abs(np.linspace(mn, mx, D, dtype=np.float32)).astype(np.float32)


def build_fft(nb):
    b = Bld()
    C = {k: b.din(k, list(v.shape), BF16 if v.dtype != np.float32 else F32) for k, v in fft_consts().items()}
    zX = [b.din("zX%d" % i, [64, 256, 128], BF16) for i in range(nb)]
    x0X = [b.din("x0X%d" % i, [64, 256, 128], BF16) for i in range(nb)]
    zemb = [b.din("zemb%d" % i, [33, NF], F32) for i in range(nb)]
    tneg = [b.din("tneg%d" % i, [128, 128], F32) for i in range(nb)]
    w1 = b.din("w1", [33, 64], F32)
    w2 = b.din("w2", [64, 64], F32)
    w3p = b.din("w3p", [64, 2, 128], F32)
    pp = b.din("pp", [128, 3, 2], F32)
    w4x = b.din("w4x", [128, 256], F32)
    brow = b.din("brow", [1, 256], F32)
    drow = b.din("drow", [128, 256], F32)
    oX = [b.dout("oX%d" % i, [64, 256, 128], BF16) for i in range(nb)]
    ssq = b.dout("ssq", [nb, 256], F32)

    cs = {}
    for k, ap in C.items():
        cs[k] = b.sb("c_" + k, list(ap.shape), ap.dtype)
        b.dma(cs[k][:], ap, w=["c_" + k])
    FrFi, Fr, Fi, nFi, GrGi, nGiGr, nGi, tw8 = (cs[k] for k in ("FrFi", "Fr", "Fi", "nFi", "GrGi", "nGiGr", "nGi", "tw8"))
    CK = ["c_" + k for k in C]
    w1s = b.sb("w1s", [33, 64], F32)
    w2s = b.sb("w2s", [64, 64], F32)
    w3s = b.sb("w3s", [64, 2, 128], F32)
    pps = b.sb("pps", [128, 3, 2], F32)
    w4f = b.sb("w4f", [128, 256], F32)
    w4b = b.sb("w4b", [128, 256], BF16)
    brs = b.sb("brs", [1, 256], F32)
    drs = b.sb("drs", [128, 256], F32)
    ones = b.sb("ones", [128, 128], F32)
    for dst, src, k in ((w1s, w1, "w1s"), (w2s, w2, "w2s"), (w3s, w3p, "w3s"), (pps, pp, "pps"), (w4f, w4x, "w4f"),
                        (brs, brow, "brs"), (drs, drow, "drs")):
        b.dma(dst[:], src, w=[k])
    b.copy("dve", w4b[:], w4f[:], r=["w4f"], w=["w4b"])
    f3 = b.sb("f3", [128, 3, 2], F32)
    b.tt("dve", f3[:, :, 1], pps[:, :, 0], pps[:, :, 1], ALU.mult, r=["pps"], w=["f3"])
    b.ts("dve", f3[:, :, 1], f3[:, :, 1], 1.0 / 3.0, None, ALU.mult, r=["f3"], w=["f3"])
    b.ts("dve", f3[:, :, 0], pps[:, :, 0], 1.0 / 3.0, None, ALU.mult, r=["pps", "f3"], w=["f3"])
    b.memset("pool", ones[:], 1.0, w=["ones"])

    H3 = b.sb("H3", [128, 128, 128], BF16)
    zt = [b.sb("zt%d" % i, [33, 512], F32) for i in range(2)]
    ha = [b.sb("ha%d" % i, [64, 512], F32) for i in range(2)]
    hb2 = [b.sb("hb%d" % i, [64, 512], F32) for i in range(2)]
    vt = b.sb("vt", [128, 512], F32)
    vt2 = b.sb("vt2", [128, 512], F32)
    tns = b.sb("tns", [128, 128], F32)
    filt = b.sb("filt", [128, GC, 128], F32)
    filtb = b.sb("filtb", [128, GC, 128], BF16)
    hfr = b.sb("hfr", [128, GC, 128], F32)
    hfi = b.sb("hfi", [128, GC, 128], F32)
    stg = b.sb("stg", [128, 8, 2, 128], F32)
    tmpa = b.sb("tmpa", [128, 8, 128], F32)
    tmpb = b.sb("tmpb", [128, 8, 128], F32)
    Ypr = b.sb("Ypr", [128, GC, 128], BF16)
    Ypi = b.sb("Ypi", [128, GC, 128], BF16)
    Pr = b.sb("Pr", [128, GC, 128], BF16)
    Pi = b.sb("Pi", [128, GC, 128], BF16)
    zg = b.sb("zg", [64, GC, 128], BF16)
    xg = b.sb("xg", [64, GC, 128], BF16)
    og = b.sb("og", [64, GC, 128], BF16)
    win = [b.sb("win%d" % i, [128, 8, GC], F32) for i in range(2)]
    part = b.sb("part", [128, GC], F32)
    srow = b.sb("srow", [1, GC], F32)
    q1 = [b.sb("q1_%d" % i, [128, 512], F32) for i in range(2)]
    q2 = [b.sb("q2_%d" % i, [128, 512], F32) for i in range(2)]
    ps = [b.psum() for _ in range(8)]
    PK = [("ps", i) for i in range(8)]
    TWO_PI = 2 * math.pi

    def sin_layer(dst_ap, p_ap, rows, li, rkeys, wkeys, r0=0, view=None):
        rs = slice(r0, r0 + rows)
        b.act(vt[rs, :], p_ap, AF.Sin, r=rkeys + ["f3"], w=["vt"], bias=f3[rs, li, 1:2], scale=f3[rs, li, 0:1])
        b.tt("dve", vt2[rs, :], vt[rs, :], vt[rs, :], ALU.mult, r=["vt"], w=["vt2"])
        b.ts("dve", vt2[rs, :], vt2[rs, :], -4.0, 3.0, ALU.mult, ALU.add, r=["vt2"], w=["vt2"])
        a0, a1 = vt2[rs, :], vt[rs, :]
        if view is not None:
            a0, a1 = view(a0), view(a1)
        b.tt("dve", dst_ap, a0, a1, ALU.mult, r=["vt", "vt2"], w=wkeys)

    def cmul_to(dr, di, ar, ai, br, bi, conj, n, rk, wk):
        b.tt("dve", tmpa[:, :n, :], ar, br, ALU.mult, r=rk, w=["tmpa"])
        b.tt("pool", tmpb[:, :n, :], ai, bi, ALU.mult, r=rk, w=["tmpb"])
        b.tt("dve", dr, tmpa[:, :n, :], tmpb[:, :n, :], ALU.add if conj else ALU.subtract, r=["tmpa", "tmpb"], w=wk)
        b.tt("pool", tmpa[:, :n, :], ar, bi, ALU.mult, r=rk + ["tmpa"], w=["tmpa"])
        b.tt("dve", tmpb[:, :n, :], ai, br, ALU.mult, r=rk + ["tmpb"], w=["tmpb"])
        b.tt("pool", di, tmpb[:, :n, :], tmpa[:, :n, :], ALU.subtract if conj else ALU.add, r=["tmpa", "tmpb"], w=wk)

    def stage1(src_fn, K, rhsA, rhsB, dr, di, conj, rkeys, wkey):
        for c0 in range(0, GC, 8):
            for j in range(8):
                c = c0 + j
                bank = ps[j // 2]
                o = bank[:, (j % 2) * 256:(j % 2) * 256 + 256]
                srcs = src_fn(c)
                if len(srcs) == 1:
                    b.mm(o, srcs[0], rhsA[:K, :], True, True, r=rkeys + CK, w=[PK[j // 2]])
                else:
                    b.mm(o, srcs[0], rhsA[:K, :], True, False, r=rkeys + CK, w=[PK[j // 2]])
                    b.mm(o, srcs[1], rhsB[:K, :], False, True, r=rkeys + CK, w=[PK[j // 2]])
            for q in range(4):
                b.copy("act", stg[:, 2 * q:2 * q + 2, :, :], ps[q][:, :].rearrange("p (c r k) -> p c r k", c=2, r=2),
                       r=[PK[q]], w=["stg"])
            cmul_to(dr[:, c0:c0 + 8, :], di[:, c0:c0 + 8, :], stg[:, :, 0, :], stg[:, :, 1, :],
                    tw8[:, 0], tw8[:, 1], conj, 8, ["stg", "c_tw8"], [wkey])

    for bi in range(nb):
        b.dma(tns[:], tneg[bi], w=["tns"])
        b.memset("pool", H3[:], 0.0, w=["H3"])
        for mt in range(NF // 512):
            z_ = zt[mt % 2]
            b.dma(z_[:], zemb[bi][:, mt * 512:(mt + 1) * 512], w=[("zt", mt % 2)])
            half = 0 if mt < 16 else 1
            p1, p2, p3 = ps[0], ps[1], ps[2]
            b.mm(p1[:64, :], w1s[:], z_[:], True, True, r=["w1s", ("zt", mt % 2)], w=[PK[0]])
            sin_layer(ha[mt % 2][:], p1[:64, :], 64, 0, [PK[0]], [("ha", mt % 2)])
            b.mm(p2[:64, :], w2s[:], ha[mt % 2][:], True, True, r=["w2s", ("ha", mt % 2)], w=[PK[1]])
            sin_layer(hb2[mt % 2][:], p2[:64, :], 64, 1, [PK[1]], [("hb", mt % 2)])
            b.mm(p3[:, :], w3s[:, half, :], hb2[mt % 2][:], True, True, r=["w3s", ("hb", mt % 2)], w=[PK[2]])
            r0 = 64 * half
            nh0 = (mt * 512) // 128
            sin_layer(H3[r0:r0 + 64, :, nh0:nh0 + 4].rearrange("p nl nh -> p nh nl"), p3[r0:r0 + 64, :], 64, 2,
                      [PK[2]], ["H3"], r0=r0, view=lambda a: a.rearrange("p (nh nl) -> p nh nl", nh=4))
        for g in range(256 // GC):
            cg = slice(g * GC, (g + 1) * GC)
            b.dma(zg[:], zX[bi][:, cg, :], w=["zg"])
            b.dma(xg[:], x0X[bi][:, cg, :], w=["xg"])
            for n0 in range(0, 128, 8):
                wv = win[(n0 // 8) % 2]
                wk_ = ("win", (n0 // 8) % 2)
                bank = ps[4 + (n0 // 8) % 2]
                bk = PK[4 + (n0 // 8) % 2]
                for j in range(8):
                    nl = n0 + j
                    b.mm(bank[:, j * GC:(j + 1) * GC], H3[:, nl, :], w4b[:, cg], True, True, r=["H3", "w4b"], w=[bk])
                    b.act(wv[:, j, :], drs[:, cg], AF.Exp, r=["drs", "tns"], w=[wk_], scale=tns[:, nl:nl + 1])
                b.tt("dve", filt[:, :, n0:n0 + 8].rearrange("p c n -> p n c"),
                     bank[:, 0:8 * GC].rearrange("p (n c) -> p n c", n=8), wv[:], ALU.mult, r=[bk, wk_], w=["filt"])
            b.act(hfr[:], filt[:], AF.Square, r=["filt"], w=["hf"])
            b.P.add("dve", lambda e: e.tensor_reduce(out=part[:], in_=hfr[:], axis=AX.X, op=ALU.add), r=["hf"], w=["part"])
            b.mm(ps[6][:, 0:GC], ones[:], part[:], True, True, r=["ones", "part"], w=[PK[6]])
            b.copy("dve", srow[:], ps[6][0:1, 0:GC], r=[PK[6]], w=["srow"])
            b.dma(ssq[bi:bi + 1, cg], srow[:], r=["srow"])
            b.act(srow[:], srow[:], AF.Ln, r=["srow"], w=["srow"], bias=EPS, scale=1.0)
            b.act(srow[:], srow[:], AF.Exp, r=["srow"], w=["srow"], scale=0.5)
            b.tt("dve", srow[:], srow[:], brs[0:1, cg], ALU.mult, r=["srow", "brs"], w=["srow"])
            b.tt("dve", filt[0:1, :, 0], filt[0:1, :, 0], srow[:], ALU.add, r=["filt", "srow"], w=["filt"])
            b.copy("act", filtb[:], filt[:], r=["filt"], w=["filtb"])
            stage1(lambda c: [filtb[:, c, :]], 128, FrFi, None, Ypr, Ypi, False, ["filtb"], "Yp")

            def stage2(sink):
                for c0 in range(0, GC, 4):
                    j = (c0 // 4) % 2
                    pr, pi = ps[4 + 2 * j], ps[5 + 2 * j]
                    yr = Ypr[:, c0:c0 + 4, :]
                    yi = Ypi[:, c0:c0 + 4, :]
                    b.mm(pr[:, :], Fr[:], yr, True, False, r=["Yp"] + CK, w=[PK[4 + 2 * j]])
                    b.mm(pr[:, :], nFi[:], yi, False, True, r=["Yp"] + CK, w=[PK[4 + 2 * j]])
                    b.mm(pi[:, :], Fi[:], yr, True, False, r=["Yp"] + CK, w=[PK[5 + 2 * j]])
                    b.mm(pi[:, :], Fr[:], yi, False, True, r=["Yp"] + CK, w=[PK[5 + 2 * j]])
                    sink(c0, j, pr, pi)

            def sink_filter(c0, j, pr, pi):
                b.copy("act", hfr[:, c0:c0 + 4, :], pr[:, :].rearrange("p (c k) -> p c k", c=4), r=[PK[4 + 2 * j]], w=["hf"])
                b.copy("dve", hfi[:, c0:c0 + 4, :], pi[:, :].rearrange("p (c k) -> p c k", c=4), r=[PK[5 + 2 * j]], w=["hf"])

            stage2(sink_filter)
            stage1(lambda c: [zg[:, c, :]], 64, FrFi, None, Ypr, Ypi, False, ["zg"], "Yp")

            def sink_data(c0, j, pr, pi):
                a_, b_ = q1[j], q2[j]
                hr = hfr[:, c0:c0 + 4, :].rearrange("p c k -> p (c k)")
                hi = hfi[:, c0:c0 + 4, :].rearrange("p c k -> p (c k)")
                dr = Pr[:, c0:c0 + 4, :].rearrange("p c k -> p (c k)")
                di = Pi[:, c0:c0 + 4, :].rearrange("p c k -> p (c k)")
                kr, ki = PK[4 + 2 * j], PK[5 + 2 * j]
                b.tt("dve", a_[:], pr[:, :], hr, ALU.mult, r=[kr, "hf"], w=[("q1", j)])
                b.tt("dve", b_[:], pi[:, :], hi, ALU.mult, r=[ki, "hf"], w=[("q2", j)])
                b.tt("pool", dr, a_[:], b_[:], ALU.subtract, r=[("q1", j), ("q2", j)], w=["P"])
                b.tt("dve", a_[:], pr[:, :], hi, ALU.mult, r=[kr, "hf", ("q1", j)], w=[("q1", j)])
                b.tt("dve", b_[:], pi[:, :], hr, ALU.mult, r=[ki, "hf", ("q2", j)], w=[("q2", j)])
                b.tt("pool", di, a_[:], b_[:], ALU.add, r=[("q1", j), ("q2", j)], w=["P"])

            stage2(sink_data)
            stage1(lambda c: [Pr[:, c, :], Pi[:, c, :]], 128, GrGi, nGiGr, Ypr, Ypi, True, ["P"], "Yp")
            for c0 in range(0, GC, 4):
                j = (c0 // 4) % 2
                po = ps[4 + j]
                b.mm(po[:64, :], FrFi[:, 0:64], Ypr[:, c0:c0 + 4, :], True, False, r=["Yp"] + CK, w=[PK[4 + j]])
                b.mm(po[:64, :], nGi[:, 0:64], Ypi[:, c0:c0 + 4, :], False, True, r=["Yp"] + CK, w=[PK[4 + j]])
                b.stt("dve", og[:, c0:c0 + 4, :].rearrange("p c k -> p (c k)"), po[:64, :], 1.0 / NF,
                      xg[:, c0:c0 + 4, :].rearrange("p c k -> p (c k)"), ALU.mult, ALU.mult, r=[PK[4 + j], "xg"], w=["og"])
            b.dma(oX[bi][:, cg, :], og[:], r=["og"])
    return b.finish()


def to_grid(a, Lseq):
    g = np.zeros((256, NF // 2), a.dtype)
    g[:, :Lseq] = a
    return np.ascontiguousarray(g.reshape(256, 64, 128).transpose(1, 0, 2))


def from_grid(o, Lseq):
    return o.transpose(1, 0, 2).reshape(256, NF // 2)[:, :Lseq]


def run_fft(x0, z, p, seqs):
    nb = len(seqs)
    nc = build_fft(nb)
    consts = fft_consts()
    deltas = hy_deltas()
    tabs = [filter_tables(Ls) for _, Ls in seqs]
    w3p = np.zeros((64, 2, 128), np.float32)
    w3p[:, 0, 0:64] = p["hy_w3"]
    w3p[:, 1, 64:128] = p["hy_w3"]
    pp = np.zeros((128, 3, 2), np.float32)
    pp[0:64, 0, 0] = p["hy_freq"]; pp[0:64, 0, 1] = p["hy_b1"]
    pp[0:64, 1, 0] = p["hy_freq"]; pp[0:64, 1, 1] = p["hy_b2"]
    pp[0:64, 2, 0] = p["hy_freq"]; pp[0:64, 2, 1] = p["hy_b3"]
    pp[64:128, 2, :] = pp[0:64, 2, :]
    maps = []
    for c in range(NCORES):
        ch = slice(256 * c, 256 * c + 256)
        m = dict(consts)
        for i, (t0, Ls) in enumerate(seqs):
            m["zX%d" % i] = to_grid(z[ch, t0:t0 + Ls], Ls)
            m["x0X%d" % i] = to_grid(x0[ch, t0:t0 + Ls], Ls)
            m["zemb%d" % i], m["tneg%d" % i] = tabs[i]
        m["w1"] = np.ascontiguousarray(p["hy_w1"]); m["w2"] = np.ascontiguousarray(p["hy_w2"]); m["w3p"] = w3p; m["pp"] = pp
        m["w4x"] = np.ascontiguousarray(np.concatenate([p["hy_w4"][:, ch], p["hy_w4"][:, D + 256 * c:D + 256 * c + 256]], 0))
        m["brow"] = np.ascontiguousarray(p["hy_bias"][ch][None, :])
        m["drow"] = np.ascontiguousarray(np.tile(deltas[ch][None, :], (128, 1)))
        maps.append(m)
    res = run(nc, maps)
    oh = np.zeros((D, T), NPBF)
    ssq = np.ones((D, 2), np.float32)
    for c in range(NCORES):
        for i, (t0, Ls) in enumerate(seqs):
            oh[256 * c:256 * c + 256, t0:t0 + Ls] = from_grid(res[c]["oX%d" % i], Ls)
            ssq[256 * c:256 * c + 256, i] = res[c]["ssq"][i]
    return oh, ssq


NCH = T // 128


def gla_consts():
    j = np.arange(128)
    le = (j[:, None] <= j[None, :]).astype(np.float32)
    ge = (j[:, None] >= j[None, :]).astype(np.float32)
    gt = (j[:, None] > j[None, :]).astype(np.float32)
    lt = (j[:, None] < j[None, :]).astype(np.float32)
    s = -1.0 / 16.0
    return {"Um": np.stack([s * le, s * ge]).astype(np.float32), "Us": np.stack([s * gt, s * lt]).astype(np.float32),
            "mask": np.stack([le, ge]).astype(np.float32)}


def build_gla():
    b = Bld()
    hT = b.din("hT", [D, T], BF16)
    wd = {n: b.din(n, [D, c], F32) for n, c in (("wq", 256), ("wk", 256), ("wv", 512), ("wa", 32), ("wG", 256), ("wgt", 512))}
    awx = b.din("awx", [2, 17, 256], F32)
    gn = b.din("gn", [128, 2], F32)
    Umd = b.din("Um", [2, 128, 128], F32)
    Usd = b.din("Us", [2, 128, 128], F32)
    mkd = b.din("mask", [2, 128, 128], F32)
    og = b.dout("og", [256, T], BF16)
    gts = b.dout("gts", [512, T], BF16)
    W = {}
    for n, c in (("wq", 256), ("wk", 256), ("wv", 512), ("wa", 32), ("wG", 256), ("wgt", 512)):
        W[n] = load_w(b, n, wd[n], D, c)
    aws = b.sb("aws", [32, 2, 256], F32)
    b.dma(aws[0:17], awx.rearrange("d r n -> r d n"), w=["aws"])
    gns = b.sb("gns", [128, 2], F32)
    b.dma(gns[:], gn, w=["gns"])
    Um = b.sb("Ums", [128, 2, 128], F32)
    Us = b.sb("Uss", [128, 2, 128], F32)
    mk = b.sb("mks", [128, 2, 128], F32)
    for dst, src, k in ((Um, Umd, "Um"), (Us, Usd, "Us"), (mk, mkd, "mk")):
        b.dma(dst[:], src.rearrange("d p n -> p d n"), w=[k])
    ones = b.sb("ones", [128, 128], F32)
    b.memset("pool", ones[:], 1.0, w=["ones"])
    aug = b.sb("aug", [32, 128], F32)
    b.memset("pool", aug[:], 1.0, w=["aug"])
    ofsD = b.nc.dram_tensor("ofsD", [128, 4, T], BF16).ap()
    S32 = [b.sb("S32_%d" % m, [128, 512], F32) for m in range(2)]
    Sb = [b.sb("Sb_%d" % m, [128, 512], BF16) for m in range(2)]
    D2 = lambda n, shape, dt: [b.sb("%s_%d" % (n, i), shape, dt) for i in range(2)]
    ofo = D2("ofo", [128, 4, 128], BF16)
    ofl = D2("ofl", [128, 4, 128], BF16)
    e1 = D2("e1", [128, 256], F32)
    sp = D2("sp", [128, 256], F32)
    eb = D2("eb", [128, 2, 128], F32)
    enb = D2("enb", [128, 2, 128], F32)
    erem = D2("erem", [128, 256], F32)
    qt = D2("qt", [128, 2, 128], BF16)
    kt = D2("kt", [128, 2, 128], BF16)
    kh = D2("kh", [128, 256], BF16)
    vb = D2("vb", [128, 512], BF16)
    am = D2("am", [128, 128], BF16)
    osum = D2("osum", [128, 4, 128], F32)
    sq = D2("sq", [128, 4, 128], F32)
    rstd = D2("rstd", [128, 128], F32)
    ogl = D2("ogl", [128, 2, 128], F32)
    ogb = D2("ogb", [128, 2, 128], BF16)
    p_qk, p_kz, p_v, p_att, p_b, p_o, p_S0, p_S1 = [b.psum() for _ in range(8)]
    p_S = [p_S0, p_S1]

    def proj_fm(dst, wn, c0, ak, h, key):
        wsb, wk_, ws_ = W[wn]
        for kc in range(KC):
            b.mm(dst, wsb[:, kc, c0:c0 + 128], h[:, kc, :], kc == 0, kc == KC - 1, r=[wk_[kc // ws_], ak], w=[key])

    A2 = ActStream(b, "hg", hT, KC)
    qkraw = [b.sb("qkraw%d" % i, [128, 4, 512], F32) for i in range(2)]
    sgT = b.sb("sgT", [128, 2, T], BF16)
    gstage = D2("gstage", [128, 4, 512], BF16)
    banks = [(p_qk, "p_qk"), (p_kz, "p_kz"), (p_v, "p_v"), (p_att, "p_att"), (p_b, "p_b"), (p_o, "p_o")]
    for ti, (t0, n) in enumerate(TT):
        hg, hgk = A2.load(ti, t0, n)
        for m in range(6):
            pb, pk = banks[m]
            wn, c0 = ("wG", m * 128) if m < 2 else ("wgt", (m - 2) * 128)
            wsb, wk_, ws_ = W[wn]
            for kc in range(KC):
                b.mm(pb[:, :n], wsb[:, kc, c0:c0 + 128], hg[:, kc, :n], kc == 0, kc == KC - 1, r=[wk_[kc // ws_], hgk], w=[pk])
            if m < 2:
                b.act(sgT[:, m, t0:t0 + n], pb[:, :n], AF.Silu, r=[pk], w=[("sgT", ti)])
            else:
                b.act(gstage[ti % 2][:, m - 2, :n], pb[:, :n], AF.Sigmoid, r=[pk], w=[("gstage", ti % 2)])
        b.dma(gts[:, t0:t0 + n].rearrange("(c p) n -> p c n", p=128), gstage[ti % 2][:, :, :n], r=[("gstage", ti % 2)])

    def group_proj(gi, t0, n):
        hg, hgk = A2.load(gi, t0, n)
        for m in range(4):
            wn, c0 = ("wq", m * 128) if m < 2 else ("wk", (m - 2) * 128)
            wsb, wk_, ws_ = W[wn]
            for kc in range(KC):
                b.mm(p_qk[:, :n], wsb[:, kc, c0:c0 + 128], hg[:, kc, :n], kc == 0, kc == KC - 1, r=[wk_[kc // ws_], hgk], w=["p_qk"])
            b.copy("act" if m % 2 else "dve", qkraw[gi % 2][:, m, :n], p_qk[:, :n], r=["p_qk"], w=[("qkraw", gi % 2)])
        return hg, hgk

    def step(ci, d, it, gi, hg, hgk, off):
        i2 = it % 2
        t0 = ci * 128
        K2 = lambda n: (n, i2)
        h, ak = hg[:, :, off:off + 128], hgk
        qk_ = qkraw[gi % 2]
        qkk = ("qkraw", gi % 2)
        wsb, wk_, ws_ = W["wk"]
        for kc in range(KC):
            b.mm(p_kz[:, 0:256], h[:, kc, :], wsb[:, kc, :], kc == 0, kc == KC - 1, r=[wk_[kc // ws_], ak], w=["p_kz"])
        wsb, wk_, ws_ = W["wv"]
        for kc in range(KC):
            b.mm(p_v[:, :], h[:, kc, :], wsb[:, kc, :], kc == 0, kc == KC - 1, r=[wk_[kc // ws_], ak], w=["p_v"])
        wsb, wk_, ws_ = W["wa"]
        for kc in range(KC):
            b.mm(p_att[0:16, 128:256], wsb[:, kc, d * 16:(d + 1) * 16], h[:, kc, :], kc == 0, kc == KC - 1,
                 r=[wk_[kc // ws_], ak], w=["p_att"])
        b.copy("dve", aug[0:16, :], p_att[0:16, 128:256], r=["p_att"], w=["aug"])
        b.mm(p_kz[:, 256:512], aug[0:17, :], aws[0:17, d, :], True, True, r=["aug", "aws"], w=["p_kz"])
        b.act(e1[i2][:], p_kz[:, 256:512], AF.Exp, r=["p_kz"], w=[K2("e1")], scale=-1.0)
        b.act(sp[i2][:], e1[i2][:], AF.Ln, r=[K2("e1")], w=[K2("sp")], bias=1.0, scale=1.0)
        for m in range(2):
            b.mm(p_b[:, m * 128:(m + 1) * 128], sp[i2][:, m * 128:(m + 1) * 128], Um[:, d, :], True, True,
                 r=[K2("sp"), "Um"], w=["p_b"])
        b.mm(p_b[:, 256:512], Us[:, d, :], sp[i2][:], True, True, r=[K2("sp"), "Us"], w=["p_b"])
        b.act(eb[i2][:], p_b[:, 0:256].rearrange("p (m n) -> p m n", m=2), AF.Exp, r=["p_b"], w=[K2("eb")])
        b.act(enb[i2][:], p_b[:, 0:256].rearrange("p (m n) -> p m n", m=2), AF.Exp, r=["p_b"], w=[K2("enb")], scale=-1.0)
        b.act(erem[i2][:], p_b[:, 256:512], AF.Exp, r=["p_b"], w=[K2("erem")])
        b.stt("dve", qt[i2][:], qk_[:, 0:2, off:off + 128], 0.0625, eb[i2][:], ALU.mult, ALU.mult,
              r=[qkk, K2("eb")], w=[K2("qt")])
        b.tt("pool", kt[i2][:], qk_[:, 2:4, off:off + 128], enb[i2][:], ALU.mult,
             r=[qkk, K2("enb")], w=[K2("kt")])
        b.tt("dve", kh[i2][:], p_kz[:, 0:256], erem[i2][:], ALU.mult, r=["p_kz", K2("erem")], w=[K2("kh")])
        b.copy("act", vb[i2][:], p_v[:, :], r=["p_v"], w=[K2("vb")])
        for m in range(2):
            b.mm(p_att[:, 0:128], kt[i2][:, m, :], qt[i2][:, m, :], m == 0, m == 1, r=[K2("kt"), K2("qt")], w=["p_att"])
        b.tt("dve", am[i2][:], p_att[:, 0:128], mk[:, d, :], ALU.mult, r=["p_att", "mk"], w=[K2("am")])
        for dvc in range(4):
            o_ = p_o[:, dvc * 128:(dvc + 1) * 128]
            b.mm(o_, vb[i2][:, dvc * 128:(dvc + 1) * 128], am[i2][:], True, False, r=[K2("vb"), K2("am")], w=["p_o"])
            for m in range(2):
                b.mm(o_, Sb[m][:, dvc * 128:(dvc + 1) * 128], qt[i2][:, m, :], False, m == 1, r=[("Sb", m), K2("qt")], w=["p_o"])
        last = 127 if d == 0 else 0
        for m in range(2):
            b.mm(p_S[m][:, :], kh[i2][:, m * 128:(m + 1) * 128], vb[i2][:], True, True, r=[K2("kh"), K2("vb")], w=[("p_S", m)])
            b.stt("dve", S32[m][:], S32[m][:], eb[i2][:, m, last:last + 1], p_S[m][:, :], ALU.mult, ALU.add,
                  r=[("S32", m), K2("eb"), ("p_S", m)], w=[("S32", m)])
            b.copy("act", Sb[m][:], S32[m][:], r=[("S32", m)], w=[("Sb", m)])
        pv = p_o[:, :].rearrange("p (c n) -> p c n", c=4)
        if d == 0:
            b.copy("dve", ofo[i2][:], pv, r=["p_o"], w=[K2("ofo")])
            b.dma(ofsD[:, :, t0:t0 + 128], ofo[i2][:], r=[K2("ofo")], w=[("ofs", ci)])
            return
        b.dma(ofl[i2][:], ofsD[:, :, t0:t0 + 128], r=[("ofs", ci)], w=[K2("ofl")])
        b.tt("dve", osum[i2][:], pv, ofl[i2][:], ALU.add, r=["p_o", K2("ofl")], w=[K2("osum")])
        b.act(sq[i2][:], osum[i2][:], AF.Square, r=[K2("osum")], w=[K2("sq")])
        for dvc in range(4):
            b.mm(p_b[:, 0:128], ones[:], sq[i2][:, dvc, :], dvc == 0, dvc == 3, r=["ones", K2("sq")], w=["p_b"])
        b.act(rstd[i2][:], p_b[:, 0:128], AF.Ln, r=["p_b"], w=[K2("rstd")], bias=EPS, scale=1.0 / 512)
        b.act(rstd[i2][:], rstd[i2][:], AF.Exp, r=[K2("rstd")], w=[K2("rstd")], scale=-0.5)
        for j in range(2):
            hc = j
            b.tt("dve", ogl[i2][:, j, :], osum[i2][:, hc, :], rstd[i2][:], ALU.mult, r=[K2("osum"), K2("rstd")], w=[K2("ogl")])
            b.stt("dve", ogb[i2][:, j, :], ogl[i2][:, j, :], gns[:, j:j + 1], sgT[:, j, t0:t0 + 128], ALU.mult, ALU.mult,
                  r=[K2("ogl"), "gns", ("sgT", min(t0 // 512, len(TT) - 1))], w=[K2("ogb")])
        b.dma(og[:, t0:t0 + 128].rearrange("(c p) n -> p c n", p=128), ogb[i2][:], r=[K2("ogb")])

    it = 0
    gi = 0
    for d in range(2):
        for m in range(2):
            b.memset("pool", S32[m][:], 0.0, w=[("S32", m)])
            b.memset("pool", Sb[m][:], 0.0, w=[("Sb", m)])
        groups = [(L, CTX, [64, 65])] + [(512 * g, 512, [4 * g + j for j in range(4)]) for g in range(16)]
        if d == 1:
            groups = [(L, CTX, [65, 64])] + [(512 * g, 512, [4 * g + j for j in range(3, -1, -1)]) for g in range(15, -1, -1)]
        for (g0, gn_, chunks) in groups:
            hg, hgk = group_proj(gi, g0, gn_)
            for ci in chunks:
                step(ci, d, it, gi, hg, hgk, ci * 128 - g0)
                it += 1
            gi += 1
    return b.finish()


def run_gla(hT, w_in_l, p):
    COL_V, COL_AF, COL_Q, COL_G, COL_GATE = 1024, 3072, 3104, 4128, 12320
    consts = gla_consts()
    nc = build_gla()
    maps = []
    for c in range(NCORES):
        hd, half = c // 2, c % 2
        awx = np.stack([np.concatenate([p["gla_aw_f"][:, 256 * hd:256 * hd + 256], p["gla_ab_f"][None, 256 * hd:256 * hd + 256]], 0),
                        np.concatenate([p["gla_aw_b"][:, 256 * hd:256 * hd + 256], p["gla_ab_b"][None, 256 * hd:256 * hd + 256]], 0)])
        v0 = COL_V + 512 * hd
        vcols = np.concatenate([np.arange(v0 + 256 * half, v0 + 256 * half + 256), np.arange(v0 + 256 * (1 - half), v0 + 256 * (1 - half) + 256)])
        m = dict(consts)
        m.update({"hT": hT,
                  "wq": np.ascontiguousarray(w_in_l[:, COL_Q + 256 * hd:COL_Q + 256 * hd + 256]),
                  "wk": np.ascontiguousarray(w_in_l[:, 256 * hd:256 * hd + 256]),
                  "wv": np.ascontiguousarray(w_in_l[:, vcols]),
                  "wa": np.ascontiguousarray(w_in_l[:, COL_AF:COL_AF + 32]),
                  "wG": np.ascontiguousarray(w_in_l[:, COL_G + 256 * c:COL_G + 256 * c + 256]),
                  "wgt": np.ascontiguousarray(np.concatenate([w_in_l[:, COL_GATE + 256 * c:COL_GATE + 256 * c + 256],
                                                              w_in_l[:, COL_GATE + D + 256 * c:COL_GATE + D + 256 * c + 256]], 1)),
                  "awx": np.ascontiguousarray(awx.astype(np.float32)),
                  "gn": np.ascontiguousarray(p["gla_norm_g"][256 * half:256 * half + 256].reshape(2, 128).T)})
        maps.append(m)
    res = run(nc, maps)
    ogT = _gather_cols(res, "og", 256)
    gates_sh = [res[c]["gts"] for c in range(NCORES)]
    return ogT, gates_sh


def build_moe(ntiles):
    b = Bld()
    nc = b.nc
    C = 512 * ntiles
    TTm = [(i * 512, 512) for i in range(ntiles)]
    hT = b.din("hT", [D, C], BF16)
    gate = b.din("gate", [128, C], F32)
    wg = b.din("wg", [D, DFE], F32)
    wu = b.din("wu", [D, DFE], F32)
    wdn = b.din("wdn", [DFE, D], F32)
    y = b.dout("y", [D, C], BF16)
    NB1, NB2, KD = DFE // 256, D // 256, DFE // 128
    wgS = nc.dram_tensor("wgS", [NB1, 128, KC * 256], BF16).ap()
    wuS = nc.dram_tensor("wuS", [NB1, 128, KC * 256], BF16).ap()
    wdS = nc.dram_tensor("wdS", [NB2, 128, KD * 256], BF16).ap()
    stg = [b.sb("stg%d" % i, [128, KD * 256], BF16) for i in range(2)]
    si = 0
    for (src, dst, nblk, kcn) in ((wg, wgS, NB1, KC), (wu, wuS, NB1, KC), (wdn, wdS, NB2, KD)):
        for blk in range(nblk):
            s_ = stg[si % 2]
            sk = ("stg", si % 2)
            for k0 in range(0, kcn, 16):
                k1 = min(kcn, k0 + 16)
                b.dma(s_[:, k0 * 256:k1 * 256].rearrange("p (kc n) -> p kc n", n=256),
                      src[k0 * 128:k1 * 128, blk * 256:(blk + 1) * 256].rearrange("(kc p) n -> p kc n", p=128),
                      w=[sk], q="gq")
            b.dma(dst[blk], s_[:, :kcn * 256], r=[sk], w=[(id(dst), blk)])
            si += 1
    A = ActStream(b, "a", hT, KC)
    gs = [b.sb("gs%d" % i, [128, 512], F32) for i in range(2)]
    act = b.sb("act", [128, KD, 512], BF16)
    wgb = [b.sb("wgb%d" % i, [128, KC * 256], BF16) for i in range(2)]
    wub = [b.sb("wub%d" % i, [128, KC * 256], BF16) for i in range(2)]
    wdb = stg
    sg = [b.sb("sg%d" % i, [128, 512], F32) for i in range(2)]
    tm = [b.sb("tm%d" % i, [128, 512], F32) for i in range(2)]
    yb = [b.sb("yb%d" % i, [128, 512], BF16) for i in range(2)]
    ps = [b.psum() for _ in range(6)]
    i1 = 0
    i2 = 0
    for ti, (t0, n) in enumerate(TTm):
        a, ak = A.load(ti, t0, n)
        g_ = gs[ti % 2]
        b.dma(g_[:, :n], gate[:, t0:t0 + n], w=[("gs", ti % 2)])
        for fb in range(NB1):
            j = fb % 2
            b.dma(wgb[j][:], wgS[fb], r=[(id(wgS), fb)], w=[("wgb", j)])
            b.dma(wub[j][:], wuS[fb], r=[(id(wuS), fb)], w=[("wub", j)])
            wgv = wgb[j][:].rearrange("p (kc n) -> p kc n", n=256)
            wuv = wub[j][:].rearrange("p (kc n) -> p kc n", n=256)
            for m in range(2):
                q = i1 % 2
                pg, pu = ps[2 * q], ps[2 * q + 1]
                for kc in range(KC):
                    b.mm(pg[:, :n], wgv[:, kc, m * 128:(m + 1) * 128], a[:, kc, :n], kc == 0, kc == KC - 1,
                         r=[("wgb", j), ak], w=[("ps", 2 * q)])
                for kc in range(KC):
                    b.mm(pu[:, :n], wuv[:, kc, m * 128:(m + 1) * 128], a[:, kc, :n], kc == 0, kc == KC - 1,
                         r=[("wub", j), ak], w=[("ps", 2 * q + 1)])
                b.act(sg[q][:, :n], pg[:, :n], AF.Silu, r=[("ps", 2 * q)], w=[("sg", q)])
                b.tt("dve", tm[q][:, :n], sg[q][:, :n], pu[:, :n], ALU.mult, r=[("sg", q), ("ps", 2 * q + 1)], w=[("tm", q)])
                b.tt("pool", act[:, fb * 2 + m, :n], tm[q][:, :n], g_[:, :n], ALU.mult, r=[("tm", q), ("gs", ti % 2)],
                     w=[("act", fb * 2 + m)])
                i1 += 1
        for ob in range(NB2):
            j = ob % 2
            b.dma(wdb[j][:], wdS[ob], r=[(id(wdS), ob)], w=[("stg", j)])
            wdv = wdb[j][:].rearrange("p (kc n) -> p kc n", n=256)
            for m in range(2):
                q = i2 % 2
                p = ps[4 + q]
                for kc in range(KD):
                    b.mm(p[:, :n], wdv[:, kc, m * 128:(m + 1) * 128], act[:, kc, :n], kc == 0, kc == KD - 1,
                         r=[("stg", j), ("act", kc)], w=[("ps", 4 + q)])
                b.copy("act" if q else "dve", yb[q][:, :n], p[:, :n], r=[("ps", 4 + q)], w=[("yb", q)])
                b.dma(y[(ob * 2 + m) * 128:(ob * 2 + m + 1) * 128, t0:t0 + n], yb[q][:, :n], r=[("yb", q)])
                i2 += 1
    return b.finish()


def run_moe(h2, gates, wg, wu, wd):
    idx = [np.nonzero(gates[:L, e])[0] for e in range(NEXP)]
    ntiles = max(1, max((len(i) + 511) // 512 for i in idx))
    Cc = 512 * ntiles
    nc = build_moe(ntiles)
    maps = []
    for e in range(NCORES):
        hE = np.zeros((D, Cc), h2.dtype)
        hE[:, :len(idx[e])] = h2[:, idx[e]]
        gE = np.zeros((128, Cc), np.float32)
        gE[:, :len(idx[e])] = gates[idx[e], e][None, :]
        maps.append({"hT": hE, "gate": gE,
                     "wg": np.ascontiguousarray(wg[e]), "wu": np.ascontiguousarray(wu[e]), "wdn": np.ascontiguousarray(wd[e])})
    res = run(nc, maps)
    ys = []
    for e in range(NCORES):
        ye = np.zeros((D, T), res[e]["y"].dtype)
        ye[:, idx[e]] = res[e]["y"][:, :len(idx[e])]
        ys.append(ye)
    return ys


_DBG = None


def _dbg(name, arr):
    if _DBG is not None:
        _DBG(name, arr)


LAYER_KEYS = ["hy_conv_w", "hy_conv_b", "hy_w1", "hy_b1", "hy_w2", "hy_b2", "hy_w3", "hy_b3", "hy_w4", "hy_freq", "hy_bias",
              "gla_aw_f", "gla_ab_f", "gla_aw_b", "gla_ab_b", "gla_norm_g", "w_up_hy", "w_up_gla", "w_out"]


def kernel(**inp):
    inp = {k: np.asarray(v) for k, v in inp.items()}
    mod = run_mod(inp)
    xT = np.ascontiguousarray(np.concatenate([inp["x"][0].T, inp["ctx"][0].T], 1))
    h, _, _ = run_rn(xT, [], None, vec_for(mod, 0, "mix", inp["norm_mix_g"][0]), BF16, want_x=False)
    out = None
    for l in range(2):
        p = {k: inp[k][l] for k in LAYER_KEYS}
        w_in_l = inp["w_in"][l]
        _dbg("h_%d" % l, h)
        x0, z = run_hy(h, w_in_l, p["hy_conv_w"], p["hy_conv_b"])
        _dbg("x0_%d" % l, x0)
        _dbg("z_%d" % l, z)
        seqs = [(0, L), (L, CTX)] if l == 0 else [(0, L)]
        oh, ssq = run_fft(x0, z, p, seqs)
        ogT, gsh = run_gla(h, w_in_l, p)
        _dbg("og_%d" % l, ogT)
        mT = run_post(oh, ogT, gsh, p["w_up_hy"], p["w_up_gla"], ssq)
        _dbg("mT_%d" % l, mT)
        mix = run_out(mT, p["w_out"])
        _dbg("mix_%d" % l, mix)
        if l == 0:
            h2, x1, _ = run_rn(xT, [mix], F32, vec_for(mod, 0, "ffn", inp["norm_ffn_g"][0]), BF16)
            _dbg("x1", x1)
            _dbg("h2_0", h2)
            f = run_ffn_dense(h2, inp["ffn_w_gate"][0], inp["ffn_w_up"][0], inp["ffn_w_down"][0])
            _dbg("f0", f)
            h, xT, _ = run_rn(x1, [f], F32, vec_for(mod, 1, "mix", inp["norm_mix_g"][1]), BF16)
        else:
            h2, x3, gates = run_rn(xT, [mix], F32, vec_for(mod, 1, "ffn", inp["norm_ffn_g"][1]), BF16,
                                   router_w=inp["router_w"][0])
            _dbg("x3", x3)
            _dbg("h2_1", h2)
            _dbg("gates", gates)
            ys = run_moe(h2, gates, inp["exp_w_gate"][0], inp["exp_w_up"][0], inp["exp_w_down"][0])
            out, _, _ = run_rn(x3, ys, BF16, vec_for(mod, 1, "final", inp["final_norm_g"]), F32, NT=256, want_x=False)
    return np.ascontiguousarray(out[:, :L].T).reshape(1, L, D).astype(np.float32)
```

```python
import contextlib
import math
import numpy as np
import ml_dtypes
import concourse.bass as bass
import concourse.mybir as mybir
from concourse.bass_utils import run_bass_kernel_spmd

F32, BF16 = mybir.dt.float32, mybir.dt.bfloat16
AF, ALU, AX = mybir.ActivationFunctionType, mybir.AluOpType, mybir.AxisListType
NPBF = ml_dtypes.bfloat16
NCORES = 8
D = 2048
L = 8192
CTX = 256
T = L + CTX
KC = 16
EPS = 1e-6
DFF = 5504
DFE = 7168
NEXP = 8

COMPUTE = ("pe", "act", "dve", "pool")


def _is_psum_key(k):
    if isinstance(k, str):
        return k.startswith("p_") or k in ("pss", "psr")
    return isinstance(k, tuple) and k[0] in ("ps", "p_S")


class Op:
    __slots__ = ("eng", "fn", "reads", "writes", "dma", "deps", "sig", "idx", "nsem")

    def __init__(self, eng, fn, reads, writes, dma, nsem):
        self.eng, self.fn, self.reads, self.writes, self.dma = eng, fn, reads, writes, dma
        self.deps = set()
        self.sig = None
        self.nsem = nsem


class Prog:
    def __init__(self, nc, n_dma_sems=24):
        self.nc = nc
        self.ops = []
        self.n_dma_sems = n_dma_sems
        self.last_w = {}
        self.readers = {}

    def engine(self, e):
        nc = self.nc
        return {"pe": nc.tensor, "act": nc.scalar, "dve": nc.vector, "pool": nc.gpsimd,
                "sp": nc.sync, "gq": nc.gpsimd}[e]

    def add(self, eng, fn, r=(), w=(), dma=False, nsem=16):
        xr = [k for k in r if _is_psum_key(k)]
        if xr:
            w = tuple(w) + tuple(k for k in xr if k not in w)
        op = Op(eng, fn, tuple(r), tuple(w), dma, nsem)
        op.idx = len(self.ops)
        deps = op.deps
        for k in op.reads:
            lw = self.last_w.get(k)
            if lw is not None:
                deps.add(lw)
        for k in op.writes:
            lw = self.last_w.get(k)
            if lw is not None:
                deps.add(lw)
            rd = self.readers.get(k)
            if rd:
                for v in rd.values():
                    if isinstance(v, list):
                        deps.update(v)
                    else:
                        deps.add(v)
        deps.discard(op.idx)
        for k in op.writes:
            self.last_w[k] = op.idx
            self.readers[k] = {}
        for k in op.reads:
            rd = self.readers.setdefault(k, {})
            if dma:
                rd.setdefault(("dma", eng), []).append(op.idx)
            else:
                rd[eng] = op.idx
        self.ops.append(op)
        return op

    def emit(self):
        nc = self.nc
        ops = self.ops
        has_dep = [False] * len(ops)
        for op in ops:
            qe = "pool" if op.eng == "gq" else op.eng
            for d in op.deps:
                dop = ops[d]
                dq = "pool" if dop.eng == "gq" else dop.eng
                if (not dop.dma) and (not op.dma) and dq == qe and qe == "pe":
                    continue
                has_dep[d] = True
        for op in ops:
            if op.dma:
                has_dep[op.idx] = True
        with contextlib.ExitStack() as st:
            esem = {e: st.enter_context(nc.semaphore("s_" + e)) for e in COMPUTE}
            dsem = [st.enter_context(nc.semaphore("d_%d" % i)) for i in range(self.n_dma_sems)]
            ecount = {e: 0 for e in COMPUTE}
            dcount = [0] * self.n_dma_sems
            dnext = 0
            known = {q: {} for q in ("pe", "act", "dve", "pool", "sp")}
            for op in ops:
                q = "pool" if op.eng == "gq" else op.eng
                eng = self.engine(op.eng)
                need = {}
                for d in op.deps:
                    dop = ops[d]
                    if dop.sig is None:
                        continue
                    kind, sid, val = dop.sig
                    if kind == "e" and sid == q and q == "pe" and not op.dma:
                        continue
                    key = (kind, sid)
                    if need.get(key, 0) < val:
                        need[key] = val
                si = None
                if op.dma:
                    si = dnext
                    dnext = (dnext + 1) % self.n_dma_sems
                    if dcount[si] > 0:
                        key = ("d", si)
                        if need.get(key, 0) < dcount[si]:
                            need[key] = dcount[si]
                for key, val in need.items():
                    if known[q].get(key, 0) >= val:
                        continue
                    sem = esem[key[1]] if key[0] == "e" else dsem[key[1]]
                    eng.wait_ge(sem, val)
                    known[q][key] = val
                ins = op.fn(eng)
                if has_dep[op.idx]:
                    if op.dma:
                        dcount[si] += op.nsem
                        ins.then_inc(dsem[si], op.nsem)
                        op.sig = ("d", si, dcount[si])
                    else:
                        ecount[q] += 1
                        ins.then_inc(esem[q], 1)
                        op.sig = ("e", q, ecount[q])
            for si in range(self.n_dma_sems):
                if dcount[si] > known["sp"].get(("d", si), 0):
                    nc.sync.wait_ge(dsem[si], dcount[si])


class Bld:
    def __init__(self):
        self.nc = bass.Bass("TRN2", target_bir_lowering=False)
        self.P = Prog(self.nc)
        self.st = contextlib.ExitStack()
        self.nps = 0

    def din(self, name, shape, dt):
        return self.nc.dram_tensor(name, list(shape), dt, kind="ExternalInput").ap()

    def dout(self, name, shape, dt):
        return self.nc.dram_tensor(name, list(shape), dt, kind="ExternalOutput").ap()

    def sb(self, name, shape, dt):
        return self.st.enter_context(self.nc.sbuf_tensor("s_" + name, list(shape), dt))

    def psum(self):
        self.nps += 1
        return self.st.enter_context(self.nc.psum_tensor("ps%d" % self.nps, [128, 512], F32))

    def dma(self, out, in_, r=(), w=(), q="sp"):
        self.P.add(q, lambda e: e.dma_start(out=out, in_=in_), r=r, w=w, dma=True)

    def mm(self, out, lhsT, rhs, start, stop, r=(), w=()):
        self.P.add("pe", lambda e: e.matmul(out, lhsT=lhsT, rhs=rhs, start=start, stop=stop), r=r, w=w)

    def act(self, out, in_, func, r=(), w=(), **kw):
        self.P.add("act", lambda e: e.activation(out=out, in_=in_, func=func, **kw), r=r, w=w)

    def tt(self, eng, out, in0, in1, op, r=(), w=()):
        self.P.add(eng, lambda e: e.tensor_tensor(out=out, in0=in0, in1=in1, op=op), r=r, w=w)

    def ts(self, eng, out, in0, s1, s2, op0, op1=None, r=(), w=()):
        if op1 is None:
            self.P.add(eng, lambda e: e.tensor_scalar(out=out, in0=in0, scalar1=s1, scalar2=None, op0=op0), r=r, w=w)
        else:
            self.P.add(eng, lambda e: e.tensor_scalar(out=out, in0=in0, scalar1=s1, scalar2=s2, op0=op0, op1=op1), r=r, w=w)

    def stt(self, eng, out, in0, scalar, in1, op0, op1, r=(), w=()):
        eng = "dve"
        self.P.add(eng, lambda e: e.scalar_tensor_tensor(out=out, in0=in0, scalar=scalar, in1=in1, op0=op0, op1=op1), r=r, w=w)

    def copy(self, eng, out, in_, r=(), w=()):
        if eng == "act":
            self.P.add("act", lambda e: e.copy(out=out, in_=in_), r=r, w=w)
        else:
            self.P.add(eng, lambda e: e.tensor_copy(out=out, in_=in_), r=r, w=w)

    def memset(self, eng, ap, val, w=()):
        self.P.add(eng, lambda e: e.memset(ap, val), w=w)

    def finish(self):
        self.P.emit()
        self.st.close()
        return self.nc


_TRACE_LOG = None


def run(nc, in_maps):
    if _TRACE_LOG is not None:
        res = run_bass_kernel_spmd(nc, in_maps, core_ids=list(range(NCORES)), trace=True)
        _TRACE_LOG.append(res.exec_time_ns)
        print("exec_time_ns", res.exec_time_ns, flush=True)
        return res.results
    res = run_bass_kernel_spmd(nc, in_maps, core_ids=list(range(NCORES)))
    return res.results


def build_mod():
    b = Bld()
    w = b.din("w", [2, D, 1536], F32)
    bias = b.din("bias", [128, 2, 12], F32)
    cv = b.din("cv", [D, 2], F32)
    out = b.dout("mod", [2, 1536, 2], F32)
    wsb = b.sb("wsb", [128, KC, 1536], F32)
    cs = b.sb("cs", [128, KC, 2], F32)
    bs = b.sb("bs", [128, 2, 12], F32)
    res = b.sb("res", [128, 2, 12, 2], F32)
    ps = [b.psum() for _ in range(2)]
    b.dma(cs[:], cv.rearrange("(kc p) v -> p kc v", p=128), w=["cs"])
    b.dma(bs[:], bias, w=["bs"])
    b.act(cs[:], cs[:], AF.Silu, r=["cs"], w=["cs"])
    for l in range(2):
        for h in range(2):
            b.dma(wsb[:, h * 8:(h + 1) * 8, :], w[l, h * 1024:(h + 1) * 1024, :].rearrange("(kc p) n -> p kc n", p=128),
                  w=[("wsb", h)])
        for m in range(12):
            p = ps[m % 2]
            for kc in range(KC):
                b.mm(p[:, 0:2], wsb[:, kc, m * 128:(m + 1) * 128], cs[:, kc, :], kc == 0, kc == KC - 1,
                     r=[("wsb", kc // 8), "cs"], w=[("ps", m % 2)])
            b.ts("dve", res[:, l, m, :], p[:, 0:2], bs[:, l, m:m + 1], None, ALU.add,
                 r=[("ps", m % 2), "bs"], w=[("res", l)])
        b.dma(out[l].rearrange("(m p) v -> p m v", p=128), res[:, l], r=[("res", l)])
    return b.finish()


def build_rn(ntok, tiles, n_y, y_dt, h_dt, router=False, want_x=True):
    b = Bld()
    NTM = max(n for _, n, _ in tiles)
    xT = b.din("xT", [D, ntok], F32)
    ys = [b.din("y%d" % j, [D, ntok], y_dt) for j in range(n_y)]
    vec = b.din("vec", [D, 7], F32)
    hout = b.dout("h", [D, ntok], h_dt)
    xout = b.dout("xn", [D, ntok], F32) if want_x else None
    vs = b.sb("vs", [128, KC, 7], F32)
    AB = b.sb("AB", [128, KC, 4], F32)
    ones = b.sb("ones", [128, 128], F32)
    xs = [b.sb("xs%d" % i, [128, KC, NTM], F32) for i in range(2)]
    ysb = [b.sb("ysb%d" % j, [128, KC, NTM], y_dt) for j in range(n_y)]
    acc = b.sb("acc", [128, KC, NTM], F32) if n_y > 1 else None
    sq = [b.sb("sq%d" % i, [128, NTM], F32) for i in range(2)]
    rstd = b.sb("rstd", [128, NTM], F32)
    h32 = b.sb("h32", [128, KC, NTM], F32)
    hb = b.sb("hb", [128, KC, NTM], h_dt) if h_dt != F32 else None
    pss = b.psum()
    if router:
        rw = b.din("rw", [D, NEXP], F32)
        gout = b.dout("gates", [ntok, NEXP], F32)
        rws = b.sb("rws", [128, KC, NEXP], F32)
        psr = b.psum()
        lg = b.sb("lg", [128, NEXP], F32)
        t1 = b.sb("t1", [128, NEXP], F32)
        t2 = b.sb("t2", [128, NEXP], F32)
        m1 = b.sb("m1", [128, 4], F32)
        b.dma(rws[:], rw.rearrange("(kc p) e -> p kc e", p=128), w=["rws"])
    b.dma(vs[:], vec.rearrange("(kc p) v -> p kc v", p=128), w=["vs"])
    b.memset("pool", ones[:], 1.0, w=["ones"])
    for c in range(2):
        b.ts("dve", AB[:, :, 2 * c], vs[:, :, 3 + 2 * c], 1.0, None, ALU.add, r=["vs"], w=["AB"])
        b.tt("dve", AB[:, :, 2 * c], AB[:, :, 2 * c], vs[:, :, 2], ALU.mult, r=["AB", "vs"], w=["AB"])
        b.copy("dve", AB[:, :, 2 * c + 1], vs[:, :, 4 + 2 * c], r=["vs"], w=["AB"])
    for ti, (t0, n, is_ctx) in enumerate(tiles):
        x_ = xs[ti % 2]
        xk = ("xs", ti % 2)
        b.dma(x_[:, :, :n], xT[:, t0:t0 + n].rearrange("(kc p) n -> p kc n", p=128), w=[xk])
        for j in range(n_y):
            b.dma(ysb[j][:, :, :n], ys[j][:, t0:t0 + n].rearrange("(kc p) n -> p kc n", p=128), w=[("y", j)],
                  q="sp")
        gcol = 1 if is_ctx else 0
        if n_y >= 1:
            src = ysb[0]
            if n_y > 1:
                b.tt("dve", acc[:, :, :n], ysb[0][:, :, :n], ysb[1][:, :, :n], ALU.add, r=[("y", 0), ("y", 1)], w=["acc"])
                for j in range(2, n_y):
                    b.tt("pool" if j % 2 else "dve", acc[:, :, :n], acc[:, :, :n], ysb[j][:, :, :n], ALU.add,
                         r=["acc", ("y", j)], w=["acc"])
                src = acc
            for kc in range(KC):
                b.stt("dve" if kc % 2 else "pool", x_[:, kc, :n], src[:, kc, :n], vs[:, kc, gcol:gcol + 1], x_[:, kc, :n],
                      ALU.mult, ALU.add, r=["acc", ("y", 0), "vs", xk], w=[xk])
            if want_x:
                b.dma(xout[:, t0:t0 + n].rearrange("(kc p) n -> p kc n", p=128), x_[:, :, :n], r=[xk])
        for kc in range(KC):
            s = sq[kc % 2]
            b.act(s[:, :n], x_[:, kc, :n], AF.Square, r=[xk], w=[("sq", kc % 2)])
            b.mm(pss[:, :n], ones[:], s[:, :n], kc == 0, kc == KC - 1, r=["ones", ("sq", kc % 2)], w=["pss"])
        b.act(rstd[:, :n], pss[:, :n], AF.Ln, r=["pss"], w=["rstd"], bias=EPS, scale=1.0 / D)
        b.act(rstd[:, :n], rstd[:, :n], AF.Exp, r=["rstd"], w=["rstd"], scale=-0.5)
        a0 = 2 if is_ctx else 0
        for kc in range(KC):
            e = "dve" if kc % 2 else "pool"
            b.tt(e, h32[:, kc, :n], x_[:, kc, :n], rstd[:, :n], ALU.mult, r=[xk, "rstd"], w=[("h32", kc)])
            dst = h32 if hb is None else hb
            b.ts(e, dst[:, kc, :n], h32[:, kc, :n], AB[:, kc, a0:a0 + 1], AB[:, kc, a0 + 1:a0 + 2], ALU.mult, ALU.add,
                 r=[("h32", kc), "AB"], w=[("hb", kc)])
        if router:
            for kc in range(KC):
                e = "dve" if kc % 2 else "pool"
                b.ts(e, h32[:, kc, :n], h32[:, kc, :n], AB[:, kc, a0:a0 + 1], AB[:, kc, a0 + 1:a0 + 2], ALU.mult, ALU.add,
                     r=[("h32", kc), ("hb", kc), "AB"], w=[("h32", kc)])
            for s0 in range(0, n, 128):
                m = min(128, n - s0)
                for kc in range(KC):
                    b.mm(psr[:m, 0:NEXP], h32[:, kc, s0:s0 + m], rws[:, kc, :], kc == 0, kc == KC - 1,
                         r=[("h32", kc), "rws"], w=["psr"])
                b.copy("dve", lg[:m], psr[:m, 0:NEXP], r=["psr"], w=["lg"])
                V = lambda *a, **k: b.P.add("dve", *a, **k)
                V(lambda e, m=m: e.tensor_reduce(out=m1[:m, 0:1], in_=lg[:m], axis=AX.X, op=ALU.max), r=["lg"], w=["m1"])
                b.ts("dve", t1[:m], lg[:m], m1[:m, 0:1], None, ALU.is_equal, r=["lg", "m1"], w=["t1"])
                b.stt("dve", t1[:m], t1[:m], -1e30, lg[:m], ALU.mult, ALU.add, r=["t1", "lg"], w=["t1"])
                V(lambda e, m=m: e.tensor_reduce(out=m1[:m, 1:2], in_=t1[:m], axis=AX.X, op=ALU.max), r=["t1"], w=["m1"])
                b.ts("dve", t1[:m], lg[:m], m1[:m, 1:2], None, ALU.is_ge, r=["lg", "m1"], w=["t1"])
                b.ts("dve", m1[:m, 2:3], m1[:m, 0:1], -1.0, None, ALU.mult, r=["m1"], w=["m1"])
                b.act(t2[:m], lg[:m], AF.Exp, r=["lg", "m1"], w=["t2"], bias=m1[:m, 2:3], scale=1.0)
                b.tt("dve", t2[:m], t2[:m], t1[:m], ALU.mult, r=["t1", "t2"], w=["t2"])
                V(lambda e, m=m: e.tensor_reduce(out=m1[:m, 3:4], in_=t2[:m], axis=AX.X, op=ALU.add), r=["t2"], w=["m1"])
                V(lambda e, m=m: e.reciprocal(out=m1[:m, 3:4], in_=m1[:m, 3:4]), r=["m1"], w=["m1"])
                b.ts("dve", t2[:m], t2[:m], m1[:m, 3:4], None, ALU.mult, r=["t2", "m1"], w=["t2"])
                b.dma(gout[t0 + s0:t0 + s0 + m, :], t2[:m], r=["t2"])
        dst = h32 if hb is None else hb
        b.dma(hout[:, t0:t0 + n].rearrange("(kc p) n -> p kc n", p=128), dst[:, :, :n],
              r=[("hb", kc) for kc in range(KC)] + [("h32", kc) for kc in range(KC)])
    return b.finish()


def run_mod(inp):
    nc = build_mod()
    cv = np.ascontiguousarray(np.stack([inp["c"][0], inp["c_ctx"]], 1))
    maps = []
    for c in range(NCORES):
        cols = np.concatenate([np.arange(k * D + 256 * c, k * D + 256 * c + 256) for k in range(6)])
        maps.append({"w": np.ascontiguousarray(inp["ada_w"][:, :, cols]),
                     "bias": np.ascontiguousarray(inp["ada_b"][:, cols].reshape(2, 12, 128).transpose(2, 0, 1)), "cv": cv})
    res = run(nc, maps)
    mod = np.zeros((2, 6, D, 2), np.float32)
    for c in range(NCORES):
        r = res[c]["mod"].reshape(2, 6, 256, 2)
        mod[:, :, 256 * c:256 * c + 256, :] = r
    return mod


def vec_for(mod, l, which, g):
    k0 = 0 if which == "mix" else 3
    if which == "final":
        z = np.zeros(D, np.float32)
        return np.ascontiguousarray(np.stack([mod[l, 5, :, 0], mod[l, 5, :, 1], g, z, z, z, z], 1))
    if which == "mix":
        gl, gc = (mod[l - 1, 5, :, 0], mod[l - 1, 5, :, 1]) if l > 0 else (np.zeros(D, np.float32),) * 2
    else:
        gl, gc = mod[l, 2, :, 0], mod[l, 2, :, 1]
    return np.ascontiguousarray(np.stack([gl, gc, g, mod[l, k0 + 1, :, 0], mod[l, k0, :, 0],
                                          mod[l, k0 + 1, :, 1], mod[l, k0, :, 1]], 1))


def tok_cols(c):
    return np.concatenate([np.arange(1024 * c, 1024 * c + 1024), np.arange(L + 32 * c, L + 32 * c + 32)])


_RN_CACHE = {}


def run_rn(xT, ys, y_dt, vec, h_dt, router_w=None, NT=512, want_x=True):
    ntok = 1056
    tiles = [(t0, min(NT, 1024 - t0), False) for t0 in range(0, 1024, NT)] + [(1024, 32, True)]
    nc = build_rn(ntok, tiles, len(ys), y_dt, h_dt, router=router_w is not None, want_x=want_x)
    maps = []
    for c in range(NCORES):
        cols = tok_cols(c)
        m = {"xT": np.ascontiguousarray(xT[:, cols]), "vec": vec}
        for j, y in enumerate(ys):
            m["y%d" % j] = np.ascontiguousarray(y[:, cols])
        if router_w is not None:
            m["rw"] = np.ascontiguousarray(router_w)
        maps.append(m)
    res = run(nc, maps)
    h = np.zeros((D, T), res[0]["h"].dtype)
    xn = np.zeros((D, T), np.float32) if want_x else None
    gates = np.zeros((T, NEXP), np.float32) if router_w is not None else None
    for c in range(NCORES):
        cols = tok_cols(c)
        h[:, cols] = res[c]["h"]
        if want_x:
            xn[:, cols] = res[c]["xn"]
        if gates is not None:
            gates[cols] = res[c]["gates"]
    return h, xn, gates


TT = [(t0, 512) for t0 in range(0, L, 512)] + [(L, CTX)]


class ActStream:
    def __init__(self, b, name, dram, KCn, dt=BF16, nbuf=2, nmax=512):
        self.b, self.name, self.dram, self.KCn, self.nbuf = b, name, dram, KCn, nbuf
        self.bufs = [b.sb("%s_%d" % (name, i), [128, KCn, nmax], dt) for i in range(nbuf)]

    def load(self, ti, t0, n, q="sp"):
        buf = self.bufs[ti % self.nbuf]
        key = (self.name, ti % self.nbuf)
        K = self.dram.shape[0]
        full = (K // 128) * 128
        if full:
            self.b.dma(buf[:, :K // 128, :n], self.dram[0:full, t0:t0 + n].rearrange("(kc p) n -> p kc n", p=128), w=[key], q=q)
        if K > full:
            self.b.dma(buf[:K - full, K // 128, :n], self.dram[full:K, t0:t0 + n], w=[key], q=q)
        return buf, key


def load_w(b, name, dram, K, ncols):
    KCn = K // 128
    wsb = b.sb(name, [128, KCn, ncols], BF16)
    step = max(1, 8192 // ncols)
    for k0 in range(0, KCn, step):
        k1 = min(KCn, k0 + step)
        b.dma(wsb[:, k0:k1, :], dram[k0 * 128:k1 * 128, :].rearrange("(kc p) n -> p kc n", p=128), w=[(name, k0)], q="gq")
    keys = [(name, k0) for k0 in range(0, KCn, step)]
    return wsb, keys, step


def build_out():
    b = Bld()
    mT = b.din("mT", [D, T], BF16)
    w = b.din("w", [D, 256], F32)
    o = b.dout("o", [256, T], F32)
    wsb, wk, ws = load_w(b, "w", w, D, 256)
    A = ActStream(b, "a", mT, KC)
    ps = [b.psum() for _ in range(4)]
    osb = [b.sb("osb%d" % i, [128, 512], F32) for i in range(4)]
    i = 0
    for ti, (t0, n) in enumerate(TT):
        a, ak = A.load(ti, t0, n)
        for m in range(2):
            p, pk = ps[i % 4], ("ps", i % 4)
            for kc in range(KC):
                b.mm(p[:, :n], wsb[:, kc, m * 128:(m + 1) * 128], a[:, kc, :n], kc == 0, kc == KC - 1,
                     r=[wk[kc // ws], ak], w=[pk])
            b.copy("act" if i % 2 else "dve", osb[i % 4][:, :n], p[:, :n], r=[pk], w=[("osb", i % 4)])
            b.dma(o[m * 128:(m + 1) * 128, t0:t0 + n], osb[i % 4][:, :n], r=[("osb", i % 4)])
            i += 1
    return b.finish()


def build_ffa():
    b = Bld()
    hT = b.din("hT", [D, T], BF16)
    wg = b.din("wg", [D, 688], F32)
    wu = b.din("wu", [D, 688], F32)
    o = b.dout("o", [688, T], BF16)
    wgs, gk, gs = load_w(b, "wg", wg, D, 688)
    wus, uk, us = load_w(b, "wu", wu, D, 688)
    A = ActStream(b, "a", hT, KC)
    ps = [b.psum() for _ in range(6)]
    sg = [b.sb("sg%d" % i, [128, 512], F32) for i in range(3)]
    ob = [b.sb("ob%d" % i, [128, 512], BF16) for i in range(3)]
    chunks = [(m * 128, 128) for m in range(5)] + [(640, 48)]
    i = 0
    for ti, (t0, n) in enumerate(TT):
        a, ak = A.load(ti, t0, n)
        for (c0, cn) in chunks:
            j = i % 3
            pg, pu = ps[2 * j], ps[2 * j + 1]
            for kc in range(KC):
                b.mm(pg[:cn, :n], wgs[:, kc, c0:c0 + cn], a[:, kc, :n], kc == 0, kc == KC - 1, r=[gk[kc // gs], ak], w=[("ps", 2 * j)])
            for kc in range(KC):
                b.mm(pu[:cn, :n], wus[:, kc, c0:c0 + cn], a[:, kc, :n], kc == 0, kc == KC - 1, r=[uk[kc // us], ak], w=[("ps", 2 * j + 1)])
            b.act(sg[j][:cn, :n], pg[:cn, :n], AF.Silu, r=[("ps", 2 * j)], w=[("sg", j)])
            b.tt("dve", ob[j][:cn, :n], sg[j][:cn, :n], pu[:cn, :n], ALU.mult, r=[("sg", j), ("ps", 2 * j + 1)], w=[("ob", j)])
            b.dma(o[c0:c0 + cn, t0:t0 + n], ob[j][:cn, :n], r=[("ob", j)])
            i += 1
    return b.finish()


def build_ffb():
    b = Bld()
    KCn = DFF // 128
    aT = b.din("aT", [DFF, T], BF16)
    w = b.din("w", [DFF, 256], F32)
    o = b.dout("o", [256, T], F32)
    wsb, wk, ws = load_w(b, "w", w, DFF, 256)
    A = ActStream(b, "a", aT, KCn)
    ps = [b.psum() for _ in range(4)]
    osb = [b.sb("osb%d" % i, [128, 512], F32) for i in range(4)]
    i = 0
    for ti, (t0, n) in enumerate(TT):
        a, ak = A.load(ti, t0, n)
        for m in range(2):
            p, pk = ps[i % 4], ("ps", i % 4)
            for kc in range(KCn):
                b.mm(p[:, :n], wsb[:, kc, m * 128:(m + 1) * 128], a[:, kc, :n], kc == 0, kc == KCn - 1,
                     r=[wk[kc // ws], ak], w=[pk])
            b.copy("act" if i % 2 else "dve", osb[i % 4][:, :n], p[:, :n], r=[pk], w=[("osb", i % 4)])
            b.dma(o[m * 128:(m + 1) * 128, t0:t0 + n], osb[i % 4][:, :n], r=[("osb", i % 4)])
            i += 1
    return b.finish()


def build_post():
    b = Bld()
    ohT = b.din("ohT", [D, T], BF16)
    ogT = b.din("ogT", [D, T], BF16)
    gT = b.din("gT", [512, T], BF16)
    wh = b.din("wh", [D, 256], F32)
    wg = b.din("wg", [D, 256], F32)
    ssq = b.din("ssq", [D, 2], F32)
    o = b.dout("o", [256, T], BF16)
    w32 = b.sb("w32", [128, KC, 256], F32)
    whs = [b.sb("whs%d" % i, [128, KC, 256], BF16) for i in range(2)]
    sv = b.sb("sv", [128, KC, 2], F32)
    b.dma(w32[:], wh.rearrange("(kc p) n -> p kc n", p=128), w=["w32"])
    b.dma(sv[:], ssq.rearrange("(kc p) v -> p kc v", p=128), w=["sv"])
    b.act(sv[:], sv[:], AF.Ln, r=["sv"], w=["sv"], bias=EPS, scale=1.0)
    b.act(sv[:], sv[:], AF.Exp, r=["sv"], w=["sv"], scale=-0.5)
    for v in range(2):
        for kc in range(KC):
            b.ts("dve" if kc % 2 else "pool", whs[v][:, kc, :], w32[:, kc, :], sv[:, kc, v:v + 1], None, ALU.mult,
                 r=["w32", "sv"], w=[("whs", v)])
    wgs, gk, gs = load_w(b, "wg", wg, D, 256)
    AH = ActStream(b, "ah", ohT, KC)
    AG = ActStream(b, "ag", ogT, KC)
    GT = ActStream(b, "gt", gT, 4)
    ps = [b.psum() for _ in range(4)]
    t1 = [b.sb("t1%d" % i, [128, 512], F32) for i in range(2)]
    t2 = [b.sb("t2%d" % i, [128, 512], F32) for i in range(2)]
    ob = [b.sb("ob%d" % i, [128, 512], BF16) for i in range(2)]
    i = 0
    for ti, (t0, n) in enumerate(TT):
        ah, ahk = AH.load(ti, t0, n)
        ag, agk = AG.load(ti, t0, n)
        gt, gtk = GT.load(ti, t0, n)
        v = 1 if t0 >= L else 0
        for m in range(2):
            j = i % 2
            p1, p2 = ps[2 * j], ps[2 * j + 1]
            for kc in range(KC):
                b.mm(p1[:, :n], whs[v][:, kc, m * 128:(m + 1) * 128], ah[:, kc, :n], kc == 0, kc == KC - 1,
                     r=[("whs", v), ahk], w=[("ps", 2 * j)])
            for kc in range(KC):
                b.mm(p2[:, :n], wgs[:, kc, m * 128:(m + 1) * 128], ag[:, kc, :n], kc == 0, kc == KC - 1,
                     r=[gk[kc // gs], agk], w=[("ps", 2 * j + 1)])
            b.tt("dve", t1[j][:, :n], p1[:, :n], gt[:, m, :n], ALU.mult, r=[("ps", 2 * j), gtk], w=[("t1", j)])
            b.tt("dve", t2[j][:, :n], p2[:, :n], gt[:, 2 + m, :n], ALU.mult, r=[("ps", 2 * j + 1), gtk], w=[("t2", j)])
            b.tt("pool", ob[j][:, :n], t1[j][:, :n], t2[j][:, :n], ALU.add, r=[("t1", j), ("t2", j)], w=[("ob", j)])
            b.dma(o[m * 128:(m + 1) * 128, t0:t0 + n], ob[j][:, :n], r=[("ob", j)])
            i += 1
    return b.finish()


def _gather_cols(res, name, rows):
    return np.concatenate([res[c][name] for c in range(NCORES)], 0)


def run_out(mT, w_out):
    nc = build_out()
    maps = [{"mT": mT, "w": np.ascontiguousarray(w_out[:, 256 * c:256 * c + 256])} for c in range(NCORES)]
    return _gather_cols(run(nc, maps), "o", 256)


def run_ffn_dense(hT, wg, wu, wd):
    nc = build_ffa()
    maps = [{"hT": hT, "wg": np.ascontiguousarray(wg[:, 688 * c:688 * c + 688]),
             "wu": np.ascontiguousarray(wu[:, 688 * c:688 * c + 688])} for c in range(NCORES)]
    aT = _gather_cols(run(nc, maps), "o", 688)
    nc = build_ffb()
    maps = [{"aT": aT, "w": np.ascontiguousarray(wd[:, 256 * c:256 * c + 256])} for c in range(NCORES)]
    return _gather_cols(run(nc, maps), "o", 256)


def run_post(ohT, ogT, gates_sh, w_up_hy, w_up_gla, ssq):
    nc = build_post()
    maps = [{"ohT": ohT, "ogT": ogT, "gT": gates_sh[c], "wh": np.ascontiguousarray(w_up_hy[:, 256 * c:256 * c + 256]),
             "wg": np.ascontiguousarray(w_up_gla[:, 256 * c:256 * c + 256]), "ssq": ssq} for c in range(NCORES)]
    return _gather_cols(run(nc, maps), "o", 256)


def build_hy():
    b = Bld()
    hT = b.din("hT", [D, T], BF16)
    w = b.din("w", [D, 768], F32)
    cw = b.din("cw", [768, 4], F32)
    x0o = b.dout("x0", [256, T], BF16)
    zo = b.dout("z", [256, T], BF16)
    wsb, wk, ws = load_w(b, "w", w, D, 768)
    cws = b.sb("cws", [128, 6, 4], F32)
    b.dma(cws[:], cw.rearrange("(m p) v -> p m v", p=128), w=["cws"])
    u = b.sb("u", [128, 6, T], BF16)
    A = ActStream(b, "a", hT, KC)
    ps = [b.psum() for _ in range(4)]
    i = 0
    for ti, (t0, n) in enumerate(TT):
        a, ak = A.load(ti, t0, n)
        for m in range(6):
            p, pk = ps[i % 4], ("ps", i % 4)
            for kc in range(KC):
                b.mm(p[:, :n], wsb[:, kc, m * 128:(m + 1) * 128], a[:, kc, :n], kc == 0, kc == KC - 1,
                     r=[wk[kc // ws], ak], w=[pk])
            b.copy("act" if i % 2 else "dve", u[:, m, t0:t0 + n], p[:, :n], r=[pk], w=[("u", m, ti)])
            i += 1
    SEG = 1024
    segs = [(s, s + SEG, 0, L) for s in range(0, L, SEG)] + [(L, T, L, T)]
    cA = [b.sb("cA%d" % i, [128, SEG], F32) for i in range(2)]
    cB = [b.sb("cB%d" % i, [128, SEG], F32) for i in range(2)]
    ob = [b.sb("cob%d" % i, [128, SEG], BF16) for i in range(4)]
    oi = 0

    def conv(dst, dk, m, s0, s1, lo, hi, eng):
        n = s1 - s0
        tiles = range(len(TT))
        rk = [("u", m, ti) for ti in tiles if TT[ti][0] < s1 + 1 and TT[ti][0] + TT[ti][1] > s0 - 1]
        b.ts(eng, dst[:, :n], u[:, m, s0:s1], cws[:, m, 1:2], cws[:, m, 3:4], ALU.mult, ALU.add, r=rk + ["cws"], w=[dk])
        if s0 > lo:
            b.stt(eng, dst[:, :n], u[:, m, s0 - 1:s1 - 1], cws[:, m, 0:1], dst[:, :n], ALU.mult, ALU.add, r=rk + ["cws", dk], w=[dk])
        else:
            b.stt(eng, dst[:, 1:n], u[:, m, s0:s1 - 1], cws[:, m, 0:1], dst[:, 1:n], ALU.mult, ALU.add, r=rk + ["cws", dk], w=[dk])
        if s1 < hi:
            b.stt(eng, dst[:, :n], u[:, m, s0 + 1:s1 + 1], cws[:, m, 2:3], dst[:, :n], ALU.mult, ALU.add, r=rk + ["cws", dk], w=[dk])
        else:
            b.stt(eng, dst[:, :n - 1], u[:, m, s0 + 1:s1], cws[:, m, 2:3], dst[:, :n - 1], ALU.mult, ALU.add, r=rk + ["cws", dk], w=[dk])

    for si, (s0, s1, lo, hi) in enumerate(segs):
        n = s1 - s0
        for j in range(2):
            q = (2 * si + j) % 2
            conv(cA[q], ("cA", q), 2 + j, s0, s1, lo, hi, "dve")
            conv(cB[q], ("cB", q), 4 + j, s0, s1, lo, hi, "pool")
            o1 = ob[oi % 4]
            b.tt("dve", o1[:, :n], cA[q][:, :n], cB[q][:, :n], ALU.mult, r=[("cA", q), ("cB", q)], w=[("ob", oi % 4)])
            b.dma(zo[j * 128:(j + 1) * 128, s0:s1], o1[:, :n], r=[("ob", oi % 4)])
            oi += 1
            conv(cA[q], ("cA", q), j, s0, s1, lo, hi, "pool")
            o2 = ob[oi % 4]
            b.copy("act", o2[:, :n], cA[q][:, :n], r=[("cA", q)], w=[("ob", oi % 4)])
            b.dma(x0o[j * 128:(j + 1) * 128, s0:s1], o2[:, :n], r=[("ob", oi % 4)])
            oi += 1
    return b.finish()


def run_hy(hT, w_in_l, conv_w, conv_b):
    nc = build_hy()
    maps = []
    COL_HY = 6176
    for c in range(NCORES):
        cols = np.concatenate([np.arange(COL_HY + j * D + 256 * c, COL_HY + j * D + 256 * c + 256) for j in range(3)])
        ccols = cols - COL_HY
        cw = np.concatenate([conv_w[:, ccols].T, conv_b[ccols][:, None]], 1)
        maps.append({"hT": hT, "w": np.ascontiguousarray(w_in_l[:, cols]), "cw": np.ascontiguousarray(cw.astype(np.float32))})
    res = run(nc, maps)
    return _gather_cols(res, "x0", 256), _gather_cols(res, "z", 256)


NF = 16384
GC = 32
HY_EMB_BANDS = 16


def fft_consts():
    a = np.arange(128)
    th = 2 * np.pi * np.outer(a, a) / 128.0
    c, s = np.cos(th), np.sin(th)
    ph = 2 * np.pi * np.outer(a, a) / NF
    bf = lambda x: np.ascontiguousarray(x.astype(np.float32).astype(NPBF))
    tw = np.stack([np.cos(ph), -np.sin(ph)], 1)
    tw8 = np.ascontiguousarray(np.repeat(tw[:, :, None, :], 8, 2).astype(np.float32))
    return {"FrFi": bf(np.concatenate([c, -s], 1)), "Fr": bf(c), "Fi": bf(-s), "nFi": bf(s),
            "GrGi": bf(np.concatenate([c, s], 1)), "nGiGr": bf(np.concatenate([-s, c], 1)),
            "nGi": bf(-s), "tw8": tw8}


def filter_tables(Lseq):
    m = np.arange(NF)
    pos = np.where(m < NF // 2, m, NF - m).astype(np.float64)
    valid = np.where(m < NF // 2, m < Lseq, (NF - m) < Lseq) & (m != NF // 2)
    valid &= ~((m >= NF // 2) & (pos == 0))
    t = pos / max(Lseq - 1, 1)
    bands = np.linspace(1e-4, HY_EMB_BANDS - 1, HY_EMB_BANDS)
    ang = (2.0 * math.pi / Lseq) * pos[:, None] * bands[None, :]
    zemb = np.concatenate([t[:, None], np.cos(ang), -np.sin(ang)], -1).T
    zemb = np.where(valid[None, :], zemb, 0.0)
    tneg = np.where(valid, -t, -1e4).reshape(128, 128)
    return np.ascontiguousarray(zemb.astype(np.float32)), np.ascontiguousarray(tneg.astype(np.float32))


def hy_deltas():
    mn, mx = math.log(1e-2) / 1.5, math.log(1e-2) / 0.3
    return np.abs(np.linspace(mn, mx, D, dtype=np.float32)).astype(np.float32)


def build_fft(nb):
    b = Bld()
    C = {k: b.din(k, list(v.shape), BF16 if v.dtype != np.float32 else F32) for k, v in fft_consts().items()}
    zX = [b.din("zX%d" % i, [64, 256, 128], BF16) for i in range(nb)]
    x0X = [b.din("x0X%d" % i, [64, 256, 128], BF16) for i in range(nb)]
    zemb = [b.din("zemb%d" % i, [33, NF], F32) for i in range(nb)]
    tneg = [b.din("tneg%d" % i, [128, 128], F32) for i in range(nb)]
    w1 = b.din("w1", [33, 64], F32)
    w2 = b.din("w2", [64, 64], F32)
    w3p = b.din("w3p", [64, 2, 128], F32)
    pp = b.din("pp", [128, 3, 2], F32)
    w4x = b.din("w4x", [128, 256], F32)
    brow = b.din("brow", [1, 256], F32)
    drow = b.din("drow", [128, 256], F32)
    oX = [b.dout("oX%d" % i, [64, 256, 128], BF16) for i in range(nb)]
    ssq = b.dout("ssq", [nb, 256], F32)

    cs = {}
    for k, ap in C.items():
        cs[k] = b.sb("c_" + k, list(ap.shape), ap.dtype)
        b.dma(cs[k][:], ap, w=["c_" + k])
    FrFi, Fr, Fi, nFi, GrGi, nGiGr, nGi, tw8 = (cs[k] for k in ("FrFi", "Fr", "Fi", "nFi", "GrGi", "nGiGr", "nGi", "tw8"))
    CK = ["c_" + k for k in C]
    w1s = b.sb("w1s", [33, 64], F32)
    w2s = b.sb("w2s", [64, 64], F32)
    w3s = b.sb("w3s", [64, 2, 128], F32)
    pps = b.sb("pps", [128, 3, 2], F32)
    w4f = b.sb("w4f", [128, 256], F32)
    w4b = b.sb("w4b", [128, 256], BF16)
    brs = b.sb("brs", [1, 256], F32)
    drs = b.sb("drs", [128, 256], F32)
    ones = b.sb("ones", [128, 128], F32)
    for dst, src, k in ((w1s, w1, "w1s"), (w2s, w2, "w2s"), (w3s, w3p, "w3s"), (pps, pp, "pps"), (w4f, w4x, "w4f"),
                        (brs, brow, "brs"), (drs, drow, "drs")):
        b.dma(dst[:], src, w=[k])
    b.copy("dve", w4b[:], w4f[:], r=["w4f"], w=["w4b"])
    f3 = b.sb("f3", [128, 3, 2], F32)
    b.tt("dve", f3[:, :, 1], pps[:, :, 0], pps[:, :, 1], ALU.mult, r=["pps"], w=["f3"])
    b.ts("dve", f3[:, :, 1], f3[:, :, 1], 1.0 / 3.0, None, ALU.mult, r=["f3"], w=["f3"])
    b.ts("dve", f3[:, :, 0], pps[:, :, 0], 1.0 / 3.0, None, ALU.mult, r=["pps", "f3"], w=["f3"])
    b.memset("pool", ones[:], 1.0, w=["ones"])

    H3 = b.sb("H3", [128, 128, 128], BF16)
    zt = [b.sb("zt%d" % i, [33, 512], F32) for i in range(2)]
    ha = [b.sb("ha%d" % i, [64, 512], F32) for i in range(2)]
    hb2 = [b.sb("hb%d" % i, [64, 512], F32) for i in range(2)]
    vt = b.sb("vt", [128, 512], F32)
    vt2 = b.sb("vt2", [128, 512], F32)
    tns = b.sb("tns", [128, 128], F32)
    filt = b.sb("filt", [128, GC, 128], F32)
    filtb = b.sb("filtb", [128, GC, 128], BF16)
    hfr = b.sb("hfr", [128, GC, 128], F32)
    hfi = b.sb("hfi", [128, GC, 128], F32)
    stg = b.sb("stg", [128, 8, 2, 128], F32)
    tmpa = b.sb("tmpa", [128, 8, 128], F32)
    tmpb = b.sb("tmpb", [128, 8, 128], F32)
    Ypr = b.sb("Ypr", [128, GC, 128], BF16)
    Ypi = b.sb("Ypi", [128, GC, 128], BF16)
    Pr = b.sb("Pr", [128, GC, 128], BF16)
    Pi = b.sb("Pi", [128, GC, 128], BF16)
    zg = b.sb("zg", [64, GC, 128], BF16)
    xg = b.sb("xg", [64, GC, 128], BF16)
    og = b.sb("og", [64, GC, 128], BF16)
    win = [b.sb("win%d" % i, [128, 8, GC], F32) for i in range(2)]
    part = b.sb("part", [128, GC], F32)
    srow = b.sb("srow", [1, GC], F32)
    q1 = [b.sb("q1_%d" % i, [128, 512], F32) for i in range(2)]
    q2 = [b.sb("q2_%d" % i, [128, 512], F32) for i in range(2)]
    ps = [b.psum() for _ in range(8)]
    PK = [("ps", i) for i in range(8)]
    TWO_PI = 2 * math.pi

    def sin_layer(dst_ap, p_ap, rows, li, rkeys, wkeys, r0=0, view=None):
        rs = slice(r0, r0 + rows)
        b.act(vt[rs, :], p_ap, AF.Sin, r=rkeys + ["f3"], w=["vt"], bias=f3[rs, li, 1:2], scale=f3[rs, li, 0:1])
        b.tt("dve", vt2[rs, :], vt[rs, :], vt[rs, :], ALU.mult, r=["vt"], w=["vt2"])
        b.ts("dve", vt2[rs, :], vt2[rs, :], -4.0, 3.0, ALU.mult, ALU.add, r=["vt2"], w=["vt2"])
        a0, a1 = vt2[rs, :], vt[rs, :]
        if view is not None:
            a0, a1 = view(a0), view(a1)
        b.tt("dve", dst_ap, a0, a1, ALU.mult, r=["vt", "vt2"], w=wkeys)

    def cmul_to(dr, di, ar, ai, br, bi, conj, n, rk, wk):
        b.tt("dve", tmpa[:, :n, :], ar, br, ALU.mult, r=rk, w=["tmpa"])
        b.tt("pool", tmpb[:, :n, :], ai, bi, ALU.mult, r=rk, w=["tmpb"])
        b.tt("dve", dr, tmpa[:, :n, :], tmpb[:, :n, :], ALU.add if conj else ALU.subtract, r=["tmpa", "tmpb"], w=wk)
        b.tt("pool", tmpa[:, :n, :], ar, bi, ALU.mult, r=rk + ["tmpa"], w=["tmpa"])
        b.tt("dve", tmpb[:, :n, :], ai, br, ALU.mult, r=rk + ["tmpb"], w=["tmpb"])
        b.tt("pool", di, tmpb[:, :n, :], tmpa[:, :n, :], ALU.subtract if conj else ALU.add, r=["tmpa", "tmpb"], w=wk)

    def stage1(src_fn, K, rhsA, rhsB, dr, di, conj, rkeys, wkey):
        for c0 in range(0, GC, 8):
            for j in range(8):
                c = c0 + j
                bank = ps[j // 2]
                o = bank[:, (j % 2) * 256:(j % 2) * 256 + 256]
                srcs = src_fn(c)
                if len(srcs) == 1:
                    b.mm(o, srcs[0], rhsA[:K, :], True, True, r=rkeys + CK, w=[PK[j // 2]])
                else:
                    b.mm(o, srcs[0], rhsA[:K, :], True, False, r=rkeys + CK, w=[PK[j // 2]])
                    b.mm(o, srcs[1], rhsB[:K, :], False, True, r=rkeys + CK, w=[PK[j // 2]])
            for q in range(4):
                b.copy("act", stg[:, 2 * q:2 * q + 2, :, :], ps[q][:, :].rearrange("p (c r k) -> p c r k", c=2, r=2),
                       r=[PK[q]], w=["stg"])
            cmul_to(dr[:, c0:c0 + 8, :], di[:, c0:c0 + 8, :], stg[:, :, 0, :], stg[:, :, 1, :],
                    tw8[:, 0], tw8[:, 1], conj, 8, ["stg", "c_tw8"], [wkey])

    for bi in range(nb):
        b.dma(tns[:], tneg[bi], w=["tns"])
        b.memset("pool", H3[:], 0.0, w=["H3"])
        for mt in range(NF // 512):
            z_ = zt[mt % 2]
            b.dma(z_[:], zemb[bi][:, mt * 512:(mt + 1) * 512], w=[("zt", mt % 2)])
            half = 0 if mt < 16 else 1
            p1, p2, p3 = ps[0], ps[1], ps[2]
            b.mm(p1[:64, :], w1s[:], z_[:], True, True, r=["w1s", ("zt", mt % 2)], w=[PK[0]])
            sin_layer(ha[mt % 2][:], p1[:64, :], 64, 0, [PK[0]], [("ha", mt % 2)])
            b.mm(p2[:64, :], w2s[:], ha[mt % 2][:], True, True, r=["w2s", ("ha", mt % 2)], w=[PK[1]])
            sin_layer(hb2[mt % 2][:], p2[:64, :], 64, 1, [PK[1]], [("hb", mt % 2)])
            b.mm(p3[:, :], w3s[:, half, :], hb2[mt % 2][:], True, True, r=["w3s", ("hb", mt % 2)], w=[PK[2]])
            r0 = 64 * half
            nh0 = (mt * 512) // 128
            sin_layer(H3[r0:r0 + 64, :, nh0:nh0 + 4].rearrange("p nl nh -> p nh nl"), p3[r0:r0 + 64, :], 64, 2,
                      [PK[2]], ["H3"], r0=r0, view=lambda a: a.rearrange("p (nh nl) -> p nh nl", nh=4))
        for g in range(256 // GC):
            cg = slice(g * GC, (g + 1) * GC)
            b.dma(zg[:], zX[bi][:, cg, :], w=["zg"])
            b.dma(xg[:], x0X[bi][:, cg, :], w=["xg"])
            for n0 in range(0, 128, 8):
                wv = win[(n0 // 8) % 2]
                wk_ = ("win", (n0 // 8) % 2)
                bank = ps[4 + (n0 // 8) % 2]
                bk = PK[4 + (n0 // 8) % 2]
                for j in range(8):
                    nl = n0 + j
                    b.mm(bank[:, j * GC:(j + 1) * GC], H3[:, nl, :], w4b[:, cg], True, True, r=["H3", "w4b"], w=[bk])
                    b.act(wv[:, j, :], drs[:, cg], AF.Exp, r=["drs", "tns"], w=[wk_], scale=tns[:, nl:nl + 1])
                b.tt("dve", filt[:, :, n0:n0 + 8].rearrange("p c n -> p n c"),
                     bank[:, 0:8 * GC].rearrange("p (n c) -> p n c", n=8), wv[:], ALU.mult, r=[bk, wk_], w=["filt"])
            b.act(hfr[:], filt[:], AF.Square, r=["filt"], w=["hf"])
            b.P.add("dve", lambda e: e.tensor_reduce(out=part[:], in_=hfr[:], axis=AX.X, op=ALU.add), r=["hf"], w=["part"])
            b.mm(ps[6][:, 0:GC], ones[:], part[:], True, True, r=["ones", "part"], w=[PK[6]])
            b.copy("dve", srow[:], ps[6][0:1, 0:GC], r=[PK[6]], w=["srow"])
            b.dma(ssq[bi:bi + 1, cg], srow[:], r=["srow"])
            b.act(srow[:], srow[:], AF.Ln, r=["srow"], w=["srow"], bias=EPS, scale=1.0)
            b.act(srow[:], srow[:], AF.Exp, r=["srow"], w=["srow"], scale=0.5)
            b.tt("dve", srow[:], srow[:], brs[0:1, cg], ALU.mult, r=["srow", "brs"], w=["srow"])
            b.tt("dve", filt[0:1, :, 0], filt[0:1, :, 0], srow[:], ALU.add, r=["filt", "srow"], w=["filt"])
            b.copy("act", filtb[:], filt[:], r=["filt"], w=["filtb"])
            stage1(lambda c: [filtb[:, c, :]], 128, FrFi, None, Ypr, Ypi, False, ["filtb"], "Yp")

            def stage2(sink):
                for c0 in range(0, GC, 4):
                    j = (c0 // 4) % 2
                    pr, pi = ps[4 + 2 * j], ps[5 + 2 * j]
                    yr = Ypr[:, c0:c0 + 4, :]
                    yi = Ypi[:, c0:c0 + 4, :]
                    b.mm(pr[:, :], Fr[:], yr, True, False, r=["Yp"] + CK, w=[PK[4 + 2 * j]])
                    b.mm(pr[:, :], nFi[:], yi, False, True, r=["Yp"] + CK, w=[PK[4 + 2 * j]])
                    b.mm(pi[:, :], Fi[:], yr, True, False, r=["Yp"] + CK, w=[PK[5 + 2 * j]])
                    b.mm(pi[:, :], Fr[:], yi, False, True, r=["Yp"] + CK, w=[PK[5 + 2 * j]])
                    sink(c0, j, pr, pi)

            def sink_filter(c0, j, pr, pi):
                b.copy("act", hfr[:, c0:c0 + 4, :], pr[:, :].rearrange("p (c k) -> p c k", c=4), r=[PK[4 + 2 * j]], w=["hf"])
                b.copy("dve", hfi[:, c0:c0 + 4, :], pi[:, :].rearrange("p (c k) -> p c k", c=4), r=[PK[5 + 2 * j]], w=["hf"])

            stage2(sink_filter)
            stage1(lambda c: [zg[:, c, :]], 64, FrFi, None, Ypr, Ypi, False, ["zg"], "Yp")

            def sink_data(c0, j, pr, pi):
                a_, b_ = q1[j], q2[j]
                hr = hfr[:, c0:c0 + 4, :].rearrange("p c k -> p (c k)")
                hi = hfi[:, c0:c0 + 4, :].rearrange("p c k -> p (c k)")
                dr = Pr[:, c0:c0 + 4, :].rearrange("p c k -> p (c k)")
                di = Pi[:, c0:c0 + 4, :].rearrange("p c k -> p (c k)")
                kr, ki = PK[4 + 2 * j], PK[5 + 2 * j]
                b.tt("dve", a_[:], pr[:, :], hr, ALU.mult, r=[kr, "hf"], w=[("q1", j)])
                b.tt("dve", b_[:], pi[:, :], hi, ALU.mult, r=[ki, "hf"], w=[("q2", j)])
                b.tt("pool", dr, a_[:], b_[:], ALU.subtract, r=[("q1", j), ("q2", j)], w=["P"])
                b.tt("dve", a_[:], pr[:, :], hi, ALU.mult, r=[kr, "hf", ("q1", j)], w=[("q1", j)])
                b.tt("dve", b_[:], pi[:, :], hr, ALU.mult, r=[ki, "hf", ("q2", j)], w=[("q2", j)])
                b.tt("pool", di, a_[:], b_[:], ALU.add, r=[("q1", j), ("q2", j)], w=["P"])

            stage2(sink_data)
            stage1(lambda c: [Pr[:, c, :], Pi[:, c, :]], 128, GrGi, nGiGr, Ypr, Ypi, True, ["P"], "Yp")
            for c0 in range(0, GC, 4):
                j = (c0 // 4) % 2
                po = ps[4 + j]
                b.mm(po[:64, :], FrFi[:, 0:64], Ypr[:, c0:c0 + 4, :], True, False, r=["Yp"] + CK, w=[PK[4 + j]])
                b.mm(po[:64, :], nGi[:, 0:64], Ypi[:, c0:c0 + 4, :], False, True, r=["Yp"] + CK, w=[PK[4 + j]])
                b.stt("dve", og[:, c0:c0 + 4, :].rearrange("p c k -> p (c k)"), po[:64, :], 1.0 / NF,
                      xg[:, c0:c0 + 4, :].rearrange("p c k -> p (c k)"), ALU.mult, ALU.mult, r=[PK[4 + j], "xg"], w=["og"])
            b.dma(oX[bi][:, cg, :], og[:], r=["og"])
    return b.finish()


def to_grid(a, Lseq):
    g = np.zeros((256, NF // 2), a.dtype)
    g[:, :Lseq] = a
    return np.ascontiguousarray(g.reshape(256, 64, 128).transpose(1, 0, 2))


def from_grid(o, Lseq):
    return o.transpose(1, 0, 2).reshape(256, NF // 2)[:, :Lseq]


def run_fft(x0, z, p, seqs):
    nb = len(seqs)
    nc = build_fft(nb)
    consts = fft_consts()
    deltas = hy_deltas()
    tabs = [filter_tables(Ls) for _, Ls in seqs]
    w3p = np.zeros((64, 2, 128), np.float32)
    w3p[:, 0, 0:64] = p["hy_w3"]
    w3p[:, 1, 64:128] = p["hy_w3"]
    pp = np.zeros((128, 3, 2), np.float32)
    pp[0:64, 0, 0] = p["hy_freq"]; pp[0:64, 0, 1] = p["hy_b1"]
    pp[0:64, 1, 0] = p["hy_freq"]; pp[0:64, 1, 1] = p["hy_b2"]
    pp[0:64, 2, 0] = p["hy_freq"]; pp[0:64, 2, 1] = p["hy_b3"]
    pp[64:128, 2, :] = pp[0:64, 2, :]
    maps = []
    for c in range(NCORES):
        ch = slice(256 * c, 256 * c + 256)
        m = dict(consts)
        for i, (t0, Ls) in enumerate(seqs):
            m["zX%d" % i] = to_grid(z[ch, t0:t0 + Ls], Ls)
            m["x0X%d" % i] = to_grid(x0[ch, t0:t0 + Ls], Ls)
            m["zemb%d" % i], m["tneg%d" % i] = tabs[i]
        m["w1"] = np.ascontiguousarray(p["hy_w1"]); m["w2"] = np.ascontiguousarray(p["hy_w2"]); m["w3p"] = w3p; m["pp"] = pp
        m["w4x"] = np.ascontiguousarray(np.concatenate([p["hy_w4"][:, ch], p["hy_w4"][:, D + 256 * c:D + 256 * c + 256]], 0))
        m["brow"] = np.ascontiguousarray(p["hy_bias"][ch][None, :])
        m["drow"] = np.ascontiguousarray(np.tile(deltas[ch][None, :], (128, 1)))
        maps.append(m)
    res = run(nc, maps)
    oh = np.zeros((D, T), NPBF)
    ssq = np.ones((D, 2), np.float32)
    for c in range(NCORES):
        for i, (t0, Ls) in enumerate(seqs):
            oh[256 * c:256 * c + 256, t0:t0 + Ls] = from_grid(res[c]["oX%d" % i], Ls)
            ssq[256 * c:256 * c + 256, i] = res[c]["ssq"][i]
    return oh, ssq


NCH = T // 128


def gla_consts():
    j = np.arange(128)
    le = (j[:, None] <= j[None, :]).astype(np.float32)
    ge = (j[:, None] >= j[None, :]).astype(np.float32)
    gt = (j[:, None] > j[None, :]).astype(np.float32)
    lt = (j[:, None] < j[None, :]).astype(np.float32)
    s = -1.0 / 16.0
    return {"Um": np.stack([s * le, s * ge]).astype(np.float32), "Us": np.stack([s * gt, s * lt]).astype(np.float32),
            "mask": np.stack([le, ge]).astype(np.float32)}


def build_gla():
    b = Bld()
    hT = b.din("hT", [D, T], BF16)
    wd = {n: b.din(n, [D, c], F32) for n, c in (("wq", 256), ("wk", 256), ("wv", 512), ("wa", 32), ("wG", 256), ("wgt", 512))}
    awx = b.din("awx", [2, 17, 256], F32)
    gn = b.din("gn", [128, 2], F32)
    Umd = b.din("Um", [2, 128, 128], F32)
    Usd = b.din("Us", [2, 128, 128], F32)
    mkd = b.din("mask", [2, 128, 128], F32)
    og = b.dout("og", [256, T], BF16)
    gts = b.dout("gts", [512, T], BF16)
    W = {}
    for n, c in (("wq", 256), ("wk", 256), ("wv", 512), ("wa", 32), ("wG", 256), ("wgt", 512)):
        W[n] = load_w(b, n, wd[n], D, c)
    aws = b.sb("aws", [32, 2, 256], F32)
    b.dma(aws[0:17], awx.rearrange("d r n -> r d n"), w=["aws"])
    gns = b.sb("gns", [128, 2], F32)
    b.dma(gns[:], gn, w=["gns"])
    Um = b.sb("Ums", [128, 2, 128], F32)
    Us = b.sb("Uss", [128, 2, 128], F32)
    mk = b.sb("mks", [128, 2, 128], F32)
    for dst, src, k in ((Um, Umd, "Um"), (Us, Usd, "Us"), (mk, mkd, "mk")):
        b.dma(dst[:], src.rearrange("d p n -> p d n"), w=[k])
    ones = b.sb("ones", [128, 128], F32)
    b.memset("pool", ones[:], 1.0, w=["ones"])
    aug = b.sb("aug", [32, 128], F32)
    b.memset("pool", aug[:], 1.0, w=["aug"])
    ofsD = b.nc.dram_tensor("ofsD", [128, 4, T], BF16).ap()
    S32 = [b.sb("S32_%d" % m, [128, 512], F32) for m in range(2)]
    Sb = [b.sb("Sb_%d" % m, [128, 512], BF16) for m in range(2)]
    D2 = lambda n, shape, dt: [b.sb("%s_%d" % (n, i), shape, dt) for i in range(2)]
    ofo = D2("ofo", [128, 4, 128], BF16)
    ofl = D2("ofl", [128, 4, 128], BF16)
    e1 = D2("e1", [128, 256], F32)
    sp = D2("sp", [128, 256], F32)
    eb = D2("eb", [128, 2, 128], F32)
    enb = D2("enb", [128, 2, 128], F32)
    erem = D2("erem", [128, 256], F32)
    qt = D2("qt", [128, 2, 128], BF16)
    kt = D2("kt", [128, 2, 128], BF16)
    kh = D2("kh", [128, 256], BF16)
    vb = D2("vb", [128, 512], BF16)
    am = D2("am", [128, 128], BF16)
    osum = D2("osum", [128, 4, 128], F32)
    sq = D2("sq", [128, 4, 128], F32)
    rstd = D2("rstd", [128, 128], F32)
    ogl = D2("ogl", [128, 2, 128], F32)
    ogb = D2("ogb", [128, 2, 128], BF16)
    p_qk, p_kz, p_v, p_att, p_b, p_o, p_S0, p_S1 = [b.psum() for _ in range(8)]
    p_S = [p_S0, p_S1]

    def proj_fm(dst, wn, c0, ak, h, key):
        wsb, wk_, ws_ = W[wn]
        for kc in range(KC):
            b.mm(dst, wsb[:, kc, c0:c0 + 128], h[:, kc, :], kc == 0, kc == KC - 1, r=[wk_[kc // ws_], ak], w=[key])

    A2 = ActStream(b, "hg", hT, KC)
    qkraw = [b.sb("qkraw%d" % i, [128, 4, 512], F32) for i in range(2)]
    sgT = b.sb("sgT", [128, 2, T], BF16)
    gstage = D2("gstage", [128, 4, 512], BF16)
    banks = [(p_qk, "p_qk"), (p_kz, "p_kz"), (p_v, "p_v"), (p_att, "p_att"), (p_b, "p_b"), (p_o, "p_o")]
    for ti, (t0, n) in enumerate(TT):
        hg, hgk = A2.load(ti, t0, n)
        for m in range(6):
            pb, pk = banks[m]
            wn, c0 = ("wG", m * 128) if m < 2 else ("wgt", (m - 2) * 128)
            wsb, wk_, ws_ = W[wn]
            for kc in range(KC):
                b.mm(pb[:, :n], wsb[:, kc, c0:c0 + 128], hg[:, kc, :n], kc == 0, kc == KC - 1, r=[wk_[kc // ws_], hgk], w=[pk])
            if m < 2:
                b.act(sgT[:, m, t0:t0 + n], pb[:, :n], AF.Silu, r=[pk], w=[("sgT", ti)])
            else:
                b.act(gstage[ti % 2][:, m - 2, :n], pb[:, :n], AF.Sigmoid, r=[pk], w=[("gstage", ti % 2)])
        b.dma(gts[:, t0:t0 + n].rearrange("(c p) n -> p c n", p=128), gstage[ti % 2][:, :, :n], r=[("gstage", ti % 2)])

    def group_proj(gi, t0, n):
        hg, hgk = A2.load(gi, t0, n)
        for m in range(4):
            wn, c0 = ("wq", m * 128) if m < 2 else ("wk", (m - 2) * 128)
            wsb, wk_, ws_ = W[wn]
            for kc in range(KC):
                b.mm(p_qk[:, :n], wsb[:, kc, c0:c0 + 128], hg[:, kc, :n], kc == 0, kc == KC - 1, r=[wk_[kc // ws_], hgk], w=["p_qk"])
            b.copy("act" if m % 2 else "dve", qkraw[gi % 2][:, m, :n], p_qk[:, :n], r=["p_qk"], w=[("qkraw", gi % 2)])
        return hg, hgk

    def stageA(ci, d, it, gi, hg, hgk, off):
        i2 = it % 2
        t0 = ci * 128
        K2 = lambda n: (n, i2)
        h, ak = hg[:, :, off:off + 128], hgk
        qk_ = qkraw[gi % 2]
        qkk = ("qkraw", gi % 2)
        wsb, wk_, ws_ = W["wk"]
        for kc in range(KC):
            b.mm(p_kz[:, 0:256], h[:, kc, :], wsb[:, kc, :], kc == 0, kc == KC - 1, r=[wk_[kc // ws_], ak], w=["p_kz"])
        wsb, wk_, ws_ = W["wv"]
        for kc in range(KC):
            b.mm(p_v[:, :], h[:, kc, :], wsb[:, kc, :], kc == 0, kc == KC - 1, r=[wk_[kc // ws_], ak], w=["p_v"])
        wsb, wk_, ws_ = W["wa"]
        for kc in range(KC):
            b.mm(p_kz[0:16, 256:384], wsb[:, kc, d * 16:(d + 1) * 16], h[:, kc, :], kc == 0, kc == KC - 1,
                 r=[wk_[kc // ws_], ak], w=["p_kz"])
        b.copy("dve", aug[0:16, :], p_kz[0:16, 256:384], r=["p_kz"], w=["aug"])
        b.mm(p_kz[:, 256:512], aug[0:17, :], aws[0:17, d, :], True, True, r=["aug", "aws"], w=["p_kz"])
        b.act(e1[i2][:], p_kz[:, 256:512], AF.Exp, r=["p_kz"], w=[K2("e1")], scale=-1.0)
        b.act(sp[i2][:], e1[i2][:], AF.Ln, r=[K2("e1")], w=[K2("sp")], bias=1.0, scale=1.0)
        for m in range(2):
            b.mm(p_b[:, m * 128:(m + 1) * 128], sp[i2][:, m * 128:(m + 1) * 128], Um[:, d, :], True, True,
                 r=[K2("sp"), "Um"], w=["p_b"])
        b.mm(p_b[:, 256:512], Us[:, d, :], sp[i2][:], True, True, r=[K2("sp"), "Us"], w=["p_b"])
        b.act(eb[i2][:], p_b[:, 0:256].rearrange("p (m n) -> p m n", m=2), AF.Exp, r=["p_b"], w=[K2("eb")])
        b.act(enb[i2][:], p_b[:, 0:256].rearrange("p (m n) -> p m n", m=2), AF.Exp, r=["p_b"], w=[K2("enb")], scale=-1.0)
        b.act(erem[i2][:], p_b[:, 256:512], AF.Exp, r=["p_b"], w=[K2("erem")])
        b.stt("dve", qt[i2][:], qk_[:, 0:2, off:off + 128], 0.0625, eb[i2][:], ALU.mult, ALU.mult,
              r=[qkk, K2("eb")], w=[K2("qt")])
        b.tt("pool", kt[i2][:], qk_[:, 2:4, off:off + 128], enb[i2][:], ALU.mult,
             r=[qkk, K2("enb")], w=[K2("kt")])
        b.tt("dve", kh[i2][:], p_kz[:, 0:256], erem[i2][:], ALU.mult, r=["p_kz", K2("erem")], w=[K2("kh")])
        b.copy("act", vb[i2][:], p_v[:, :], r=["p_v"], w=[K2("vb")])

    def stageB(ci, d, it, gi, hg, hgk, off):
        i2 = it % 2
        t0 = ci * 128
        K2 = lambda n: (n, i2)
        for m in range(2):
            b.mm(p_att[:, 0:128], kt[i2][:, m, :], qt[i2][:, m, :], m == 0, m == 1, r=[K2("kt"), K2("qt")], w=["p_att"])
        b.tt("dve", am[i2][:], p_att[:, 0:128], mk[:, d, :], ALU.mult, r=["p_att", "mk"], w=[K2("am")])
        for dvc in range(4):
            o_ = p_o[:, dvc * 128:(dvc + 1) * 128]
            b.mm(o_, vb[i2][:, dvc * 128:(dvc + 1) * 128], am[i2][:], True, False, r=[K2("vb"), K2("am")], w=["p_o"])
            for m in range(2):
                b.mm(o_, Sb[m][:, dvc * 128:(dvc + 1) * 128], qt[i2][:, m, :], False, m == 1, r=[("Sb", m), K2("qt")], w=["p_o"])
        last = 127 if d == 0 else 0
        for m in range(2):
            b.mm(p_S[m][:, :], kh[i2][:, m * 128:(m + 1) * 128], vb[i2][:], True, True, r=[K2("kh"), K2("vb")], w=[("p_S", m)])
            b.stt("dve", S32[m][:], S32[m][:], eb[i2][:, m, last:last + 1], p_S[m][:, :], ALU.mult, ALU.add,
                  r=[("S32", m), K2("eb"), ("p_S", m)], w=[("S32", m)])
            b.copy("act", Sb[m][:], S32[m][:], r=[("S32", m)], w=[("Sb", m)])
        pv = p_o[:, :].rearrange("p (c n) -> p c n", c=4)
        if d == 0:
            b.copy("dve", ofo[i2][:], pv, r=["p_o"], w=[K2("ofo")])
            b.dma(ofsD[:, :, t0:t0 + 128], ofo[i2][:], r=[K2("ofo")], w=[("ofs", ci)])
            return
        b.dma(ofl[i2][:], ofsD[:, :, t0:t0 + 128], r=[("ofs", ci)], w=[K2("ofl")])
        b.tt("dve", osum[i2][:], pv, ofl[i2][:], ALU.add, r=["p_o", K2("ofl")], w=[K2("osum")])
        b.act(sq[i2][:], osum[i2][:], AF.Square, r=[K2("osum")], w=[K2("sq")])
        for dvc in range(4):
            b.mm(p_att[:, 128:256], ones[:], sq[i2][:, dvc, :], dvc == 0, dvc == 3, r=["ones", K2("sq")], w=["p_att"])
        b.act(rstd[i2][:], p_att[:, 128:256], AF.Ln, r=["p_att"], w=[K2("rstd")], bias=EPS, scale=1.0 / 512)
        b.act(rstd[i2][:], rstd[i2][:], AF.Exp, r=[K2("rstd")], w=[K2("rstd")], scale=-0.5)
        for j in range(2):
            hc = j
            b.tt("dve", ogl[i2][:, j, :], osum[i2][:, hc, :], rstd[i2][:], ALU.mult, r=[K2("osum"), K2("rstd")], w=[K2("ogl")])
            b.stt("dve", ogb[i2][:, j, :], ogl[i2][:, j, :], gns[:, j:j + 1], sgT[:, j, t0:t0 + 128], ALU.mult, ALU.mult,
                  r=[K2("ogl"), "gns", ("sgT", min(t0 // 512, len(TT) - 1))], w=[K2("ogb")])
        b.dma(og[:, t0:t0 + 128].rearrange("(c p) n -> p c n", p=128), ogb[i2][:], r=[K2("ogb")])

    it = 0
    gi = 0
    for d in range(2):
        for m in range(2):
            b.memset("pool", S32[m][:], 0.0, w=[("S32", m)])
            b.memset("pool", Sb[m][:], 0.0, w=[("Sb", m)])
        groups = [(L, CTX, [64, 65])] + [(512 * g, 512, [4 * g + j for j in range(4)]) for g in range(16)]
        if d == 1:
            groups = [(L, CTX, [65, 64])] + [(512 * g, 512, [4 * g + j for j in range(3, -1, -1)]) for g in range(15, -1, -1)]
        pend = None
        for (g0, gn_, chunks) in groups:
            hg, hgk = group_proj(gi, g0, gn_)
            for ci in chunks:
                args = (ci, d, it, gi, hg, hgk, ci * 128 - g0)
                stageA(*args)
                if pend is not None:
                    stageB(*pend)
                pend = args
                it += 1
            gi += 1
        stageB(*pend)
    return b.finish()


def run_gla(hT, w_in_l, p):
    COL_V, COL_AF, COL_Q, COL_G, COL_GATE = 1024, 3072, 3104, 4128, 12320
    consts = gla_consts()
    nc = build_gla()
    maps = []
    for c in range(NCORES):
        hd, half = c // 2, c % 2
        awx = np.stack([np.concatenate([p["gla_aw_f"][:, 256 * hd:256 * hd + 256], p["gla_ab_f"][None, 256 * hd:256 * hd + 256]], 0),
                        np.concatenate([p["gla_aw_b"][:, 256 * hd:256 * hd + 256], p["gla_ab_b"][None, 256 * hd:256 * hd + 256]], 0)])
        v0 = COL_V + 512 * hd
        vcols = np.concatenate([np.arange(v0 + 256 * half, v0 + 256 * half + 256), np.arange(v0 + 256 * (1 - half), v0 + 256 * (1 - half) + 256)])
        m = dict(consts)
        m.update({"hT": hT,
                  "wq": np.ascontiguousarray(w_in_l[:, COL_Q + 256 * hd:COL_Q + 256 * hd + 256]),
                  "wk": np.ascontiguousarray(w_in_l[:, 256 * hd:256 * hd + 256]),
                  "wv": np.ascontiguousarray(w_in_l[:, vcols]),
                  "wa": np.ascontiguousarray(w_in_l[:, COL_AF:COL_AF + 32]),
                  "wG": np.ascontiguousarray(w_in_l[:, COL_G + 256 * c:COL_G + 256 * c + 256]),
                  "wgt": np.ascontiguousarray(np.concatenate([w_in_l[:, COL_GATE + 256 * c:COL_GATE + 256 * c + 256],
                                                              w_in_l[:, COL_GATE + D + 256 * c:COL_GATE + D + 256 * c + 256]], 1)),
                  "awx": np.ascontiguousarray(awx.astype(np.float32)),
                  "gn": np.ascontiguousarray(p["gla_norm_g"][256 * half:256 * half + 256].reshape(2, 128).T)})
        maps.append(m)
    res = run(nc, maps)
    ogT = _gather_cols(res, "og", 256)
    gates_sh = [res[c]["gts"] for c in range(NCORES)]
    return ogT, gates_sh


def build_moe(ntiles):
    b = Bld()
    nc = b.nc
    C = 512 * ntiles
    TTm = [(i * 512, 512) for i in range(ntiles)]
    hT = b.din("hT", [D, C], BF16)
    gate = b.din("gate", [128, C], F32)
    wg = b.din("wg", [D, DFE], F32)
    wu = b.din("wu", [D, DFE], F32)
    wdn = b.din("wdn", [DFE, D], F32)
    y = b.dout("y", [D, C], BF16)
    NB1, NB2, KD = DFE // 256, D // 256, DFE // 128
    wgS = nc.dram_tensor("wgS", [NB1, 128, KC * 256], BF16).ap()
    wuS = nc.dram_tensor("wuS", [NB1, 128, KC * 256], BF16).ap()
    wdS = nc.dram_tensor("wdS", [NB2, 128, KD * 256], BF16).ap()
    stg = [b.sb("stg%d" % i, [128, KD * 256], BF16) for i in range(2)]
    si = 0
    for (src, dst, nblk, kcn) in ((wg, wgS, NB1, KC), (wu, wuS, NB1, KC), (wdn, wdS, NB2, KD)):
        for blk in range(nblk):
            s_ = stg[si % 2]
            sk = ("stg", si % 2)
            for k0 in range(0, kcn, 16):
                k1 = min(kcn, k0 + 16)
                b.dma(s_[:, k0 * 256:k1 * 256].rearrange("p (kc n) -> p kc n", n=256),
                      src[k0 * 128:k1 * 128, blk * 256:(blk + 1) * 256].rearrange("(kc p) n -> p kc n", p=128),
                      w=[sk], q="gq")
            b.dma(dst[blk], s_[:, :kcn * 256], r=[sk], w=[(id(dst), blk)])
            si += 1
    A = ActStream(b, "a", hT, KC)
    gs = [b.sb("gs%d" % i, [128, 512], F32) for i in range(2)]
    act = b.sb("act", [128, KD, 512], BF16)
    wgb = [b.sb("wgb%d" % i, [128, KC * 256], BF16) for i in range(2)]
    wub = [b.sb("wub%d" % i, [128, KC * 256], BF16) for i in range(2)]
    wdb = stg
    sg = [b.sb("sg%d" % i, [128, 512], F32) for i in range(2)]
    tm = [b.sb("tm%d" % i, [128, 512], F32) for i in range(2)]
    yb = [b.sb("yb%d" % i, [128, 512], BF16) for i in range(2)]
    ps = [b.psum() for _ in range(6)]
    i1 = 0
    i2 = 0
    for ti, (t0, n) in enumerate(TTm):
        a, ak = A.load(ti, t0, n)
        g_ = gs[ti % 2]
        b.dma(g_[:, :n], gate[:, t0:t0 + n], w=[("gs", ti % 2)])
        for fb in range(NB1):
            j = fb % 2
            b.dma(wgb[j][:], wgS[fb], r=[(id(wgS), fb)], w=[("wgb", j)])
            b.dma(wub[j][:], wuS[fb], r=[(id(wuS), fb)], w=[("wub", j)])
            wgv = wgb[j][:].rearrange("p (kc n) -> p kc n", n=256)
            wuv = wub[j][:].rearrange("p (kc n) -> p kc n", n=256)
            for m in range(2):
                q = i1 % 2
                pg, pu = ps[2 * q], ps[2 * q + 1]
                for kc in range(KC):
                    b.mm(pg[:, :n], wgv[:, kc, m * 128:(m + 1) * 128], a[:, kc, :n], kc == 0, kc == KC - 1,
                         r=[("wgb", j), ak], w=[("ps", 2 * q)])
                for kc in range(KC):
                    b.mm(pu[:, :n], wuv[:, kc, m * 128:(m + 1) * 128], a[:, kc, :n], kc == 0, kc == KC - 1,
                         r=[("wub", j), ak], w=[("ps", 2 * q + 1)])
                b.act(sg[q][:, :n], pg[:, :n], AF.Silu, r=[("ps", 2 * q)], w=[("sg", q)])
                b.tt("dve", tm[q][:, :n], sg[q][:, :n], pu[:, :n], ALU.mult, r=[("sg", q), ("ps", 2 * q + 1)], w=[("tm", q)])
                b.tt("pool", act[:, fb * 2 + m, :n], tm[q][:, :n], g_[:, :n], ALU.mult, r=[("tm", q), ("gs", ti % 2)],
                     w=[("act", fb * 2 + m)])
                i1 += 1
        for ob in range(NB2):
            j = ob % 2
            b.dma(wdb[j][:], wdS[ob], r=[(id(wdS), ob)], w=[("stg", j)])
            wdv = wdb[j][:].rearrange("p (kc n) -> p kc n", n=256)
            for m in range(2):
                q = i2 % 2
                p = ps[4 + q]
                for kc in range(KD):
                    b.mm(p[:, :n], wdv[:, kc, m * 128:(m + 1) * 128], act[:, kc, :n], kc == 0, kc == KD - 1,
                         r=[("stg", j), ("act", kc)], w=[("ps", 4 + q)])
                b.copy("act" if q else "dve", yb[q][:, :n], p[:, :n], r=[("ps", 4 + q)], w=[("yb", q)])
                b.dma(y[(ob * 2 + m) * 128:(ob * 2 + m + 1) * 128, t0:t0 + n], yb[q][:, :n], r=[("yb", q)])
                i2 += 1
    return b.finish()


def run_moe(h2, gates, wg, wu, wd):
    idx = [np.nonzero(gates[:L, e])[0] for e in range(NEXP)]
    ntiles = max(1, max((len(i) + 511) // 512 for i in idx))
    Cc = 512 * ntiles
    nc = build_moe(ntiles)
    maps = []
    for e in range(NCORES):
        hE = np.zeros((D, Cc), h2.dtype)
        hE[:, :len(idx[e])] = h2[:, idx[e]]
        gE = np.zeros((128, Cc), np.float32)
        gE[:, :len(idx[e])] = gates[idx[e], e][None, :]
        maps.append({"hT": hE, "gate": gE,
                     "wg": np.ascontiguousarray(wg[e]), "wu": np.ascontiguousarray(wu[e]), "wdn": np.ascontiguousarray(wd[e])})
    res = run(nc, maps)
    ys = []
    for e in range(NCORES):
        ye = np.zeros((D, T), res[e]["y"].dtype)
        ye[:, idx[e]] = res[e]["y"][:, :len(idx[e])]
        ys.append(ye)
    return ys


_DBG = None


def _dbg(name, arr):
    if _DBG is not None:
        _DBG(name, arr)


LAYER_KEYS = ["hy_conv_w", "hy_conv_b", "hy_w1", "hy_b1", "hy_w2", "hy_b2", "hy_w3", "hy_b3", "hy_w4", "hy_freq", "hy_bias",
              "gla_aw_f", "gla_ab_f", "gla_aw_b", "gla_ab_b", "gla_norm_g", "w_up_hy", "w_up_gla", "w_out"]


def kernel(**inp):
    inp = {k: np.asarray(v) for k, v in inp.items()}
    mod = run_mod(inp)
    xT = np.ascontiguousarray(np.concatenate([inp["x"][0].T, inp["ctx"][0].T], 1))
    h, _, _ = run_rn(xT, [], None, vec_for(mod, 0, "mix", inp["norm_mix_g"][0]), BF16, want_x=False)
    out = None
    for l in range(2):
        p = {k: inp[k][l] for k in LAYER_KEYS}
        w_in_l = inp["w_in"][l]
        _dbg("h_%d" % l, h)
        x0, z = run_hy(h, w_in_l, p["hy_conv_w"], p["hy_conv_b"])
        _dbg("x0_%d" % l, x0)
        _dbg("z_%d" % l, z)
        seqs = [(0, L), (L, CTX)] if l == 0 else [(0, L)]
        oh, ssq = run_fft(x0, z, p, seqs)
        ogT, gsh = run_gla(h, w_in_l, p)
        _dbg("og_%d" % l, ogT)
        mT = run_post(oh, ogT, gsh, p["w_up_hy"], p["w_up_gla"], ssq)
        _dbg("mT_%d" % l, mT)
        mix = run_out(mT, p["w_out"])
        _dbg("mix_%d" % l, mix)
        if l == 0:
            h2, x1, _ = run_rn(xT, [mix], F32, vec_for(mod, 0, "ffn", inp["norm_ffn_g"][0]), BF16)
            _dbg("x1", x1)
            _dbg("h2_0", h2)
            f = run_ffn_dense(h2, inp["ffn_w_gate"][0], inp["ffn_w_up"][0], inp["ffn_w_down"][0])
            _dbg("f0", f)
            h, xT, _ = run_rn(x1, [f], F32, vec_for(mod, 1, "mix", inp["norm_mix_g"][1]), BF16)
        else:
            h2, x3, gates = run_rn(xT, [mix], F32, vec_for(mod, 1, "ffn", inp["norm_ffn_g"][1]), BF16,
                                   router_w=inp["router_w"][0])
            _dbg("x3", x3)
            _dbg("h2_1", h2)
            _dbg("gates", gates)
            ys = run_moe(h2, gates, inp["exp_w_gate"][0], inp["exp_w_up"][0], inp["exp_w_down"][0])
            out, _, _ = run_rn(x3, ys, BF16, vec_for(mod, 1, "final", inp["final_norm_g"]), F32, NT=256, want_x=False)
    return np.ascontiguousarray(out[:, :L].T).reshape(1, L, D).astype(np.float32)
```
